# Optimizing a Trainium2 kernel written in Bass

```python
import math
import jax, jax.numpy as jnp
from jax import lax
import numpy as np

D_MODEL = 1024
BATCH = 4
SEQ = 8192
DEPTH = 4

GRID_W = 64
CTX_LEN = 256
N_MIXERS = 3
ROPE_BASE = 10000.0
EPS = 1e-6
Q_BLOCK = 128

DA_HEADS = 8
DA_HEAD_DIM = 64
MLA_HEADS = 16
MLA_Q_RANK = 256
MLA_KV_RANK = 128
MLA_NOPE = 64
MLA_ROPE = 32
MLA_V = 64
MLA_QK = MLA_NOPE + MLA_ROPE
GDN_HEADS = 8
GDN_DK = 128
GDN_DV = 128
GDN_CONV = 5
GDN_CHUNK = 64
N_EXPERTS = 16
EXPERT_FF = 2048
EC_FACTOR = 2

kernel_name = 'hybrid_diffattn_mla_gdn_ecmoe_prefix'

F32 = jnp.float32


def rms_norm(x, g):
    xf = x.astype(F32)
    y = xf * lax.rsqrt(jnp.mean(xf * xf, axis=-1, keepdims=True) + EPS)
    return (y * g.astype(F32)).astype(x.dtype)


def l2_norm(x):
    xf = x.astype(F32)
    return (xf * lax.rsqrt(jnp.sum(xf * xf, axis=-1, keepdims=True) + EPS)).astype(x.dtype)


def axial_rope_tables(rows, dim):
    nf = dim // 4
    inv = ROPE_BASE ** (-jnp.arange(nf, dtype=F32) / nf)
    r = jnp.repeat(jnp.arange(rows, dtype=F32), GRID_W)
    c = jnp.tile(jnp.arange(GRID_W, dtype=F32), rows)
    ang = jnp.concatenate([r[:, None] * inv, c[:, None] * inv], axis=-1)
    return jnp.cos(ang), jnp.sin(ang)


def apply_rope(x, cos, sin):
    half = x.shape[-1] // 2
    x1, x2 = x[..., :half], x[..., half:]
    cs, sn = cos[:, None, :], sin[:, None, :]
    return jnp.concatenate([x1 * cs - x2 * sn, x2 * cs + x1 * sn], axis=-1).astype(x.dtype)


def over_query_blocks(fn, *qs):
    b, n = qs[0].shape[:2]
    nb = n // Q_BLOCK
    blocks = tuple(q.reshape((b, nb, Q_BLOCK) + q.shape[2:]).swapaxes(0, 1) for q in qs)
    out = lax.map(lambda t: fn(*t), blocks)
    return out.swapaxes(0, 1).reshape((b, n) + out.shape[3:])


def softmax_core(q, k, v, scale):
    s = jnp.einsum('bqhd,bkhd->bhqk', q, k).astype(F32) * scale
    p = jax.nn.softmax(s, axis=-1)
    return jnp.einsum('bhqk,bkhe->bqhe', p.astype(v.dtype), v)


def diff_core(q1, q2, k1, k2, v, lam):
    scale = DA_HEAD_DIM ** -0.5
    s1 = jnp.einsum('bqhd,bkhd->bhqk', q1, k1).astype(F32) * scale
    s2 = jnp.einsum('bqhd,bkhd->bhqk', q2, k2).astype(F32) * scale
    p = jax.nn.softmax(s1, axis=-1) - lam * jax.nn.softmax(s2, axis=-1)
    return jnp.einsum('bhqk,bkhe->bqhe', p.astype(v.dtype), v)


def diff_attention(hc, hl, w_in, q_gain, k_gain, lam_vecs, sub_gain, w_out, lam_init, cos, sin, need_ctx):
    lv = lam_vecs.astype(F32)
    lam = jnp.exp(jnp.sum(lv[0] * lv[1])) - jnp.exp(jnp.sum(lv[2] * lv[3])) + lam_init

    def project(h, rope):
        b, n, _ = h.shape
        q, k, v = jnp.split(h @ w_in, 3, axis=-1)
        q = rms_norm(q.reshape(b, n, 2 * DA_HEADS, DA_HEAD_DIM), q_gain)
        k = rms_norm(k.reshape(b, n, 2 * DA_HEADS, DA_HEAD_DIM), k_gain)
        if rope:
            q = apply_rope(q, cos, sin)
            k = apply_rope(k, cos, sin)
        q = q.reshape(b, n, DA_HEADS, 2, DA_HEAD_DIM)
        k = k.reshape(b, n, DA_HEADS, 2, DA_HEAD_DIM)
        v = v.reshape(b, n, DA_HEADS, 2 * DA_HEAD_DIM)
        return q[..., 0, :], q[..., 1, :], k[..., 0, :], k[..., 1, :], v

    def finish(o):
        b, n = o.shape[:2]
        o = rms_norm(o, sub_gain) * (1.0 - lam_init)
        return o.reshape(b, n, -1) @ w_out

    q1c, q2c, k1c, k2c, vc = project(hc, False)
    q1l, q2l, k1l, k2l, vl = project(hl, True)
    k1 = jnp.concatenate([k1c, k1l], axis=1)
    k2 = jnp.concatenate([k2c, k2l], axis=1)
    v = jnp.concatenate([vc, vl], axis=1)
    out_l = finish(over_query_blocks(lambda a, b_: diff_core(a, b_, k1, k2, v, lam), q1l, q2l))
    out_c = finish(diff_core(q1c, q2c, k1c, k2c, vc, lam)) if need_ctx else None
    return out_c, out_l


def mla_attention(hc, hl, w_down, q_a_gain, kv_a_gain, w_uq, w_ukv, q_gain, k_gain, w_out, cos, sin, need_ctx):
    def project(h, rope):
        b, n, _ = h.shape
        lat = h @ w_down
        cq = lat[..., :MLA_Q_RANK]
        ckv = lat[..., MLA_Q_RANK:MLA_Q_RANK + MLA_KV_RANK]
        kr = lat[..., MLA_Q_RANK + MLA_KV_RANK:]
        q = (rms_norm(cq, q_a_gain) @ w_uq).reshape(b, n, MLA_HEADS, MLA_QK)
        kv = (rms_norm(ckv, kv_a_gain) @ w_ukv).reshape(b, n, MLA_HEADS, MLA_NOPE + MLA_V)
        k = jnp.concatenate([kv[..., :MLA_NOPE], jnp.broadcast_to(kr[:, :, None, :], (b, n, MLA_HEADS, MLA_ROPE))], axis=-1)
        v = kv[..., MLA_NOPE:]
        q = rms_norm(q, q_gain)
        k = rms_norm(k, k_gain)
        if rope:
            q = jnp.concatenate([q[..., :MLA_NOPE], apply_rope(q[..., MLA_NOPE:], cos, sin)], axis=-1)
            k = jnp.concatenate([k[..., :MLA_NOPE], apply_rope(k[..., MLA_NOPE:], cos, sin)], axis=-1)
        return q, k, v

    def finish(o):
        b, n = o.shape[:2]
        return o.reshape(b, n, -1) @ w_out

    scale = MLA_QK ** -0.5
    qc, kc, vc = project(hc, False)
    ql, kl, vl = project(hl, True)
    k = jnp.concatenate([kc, kl], axis=1)
    v = jnp.concatenate([vc, vl], axis=1)
    out_l = finish(over_query_blocks(lambda a: softmax_core(a, k, v, scale), ql))
    out_c = finish(softmax_core(qc, kc, vc, scale)) if need_ctx else None
    return out_c, out_l


def centred_depthwise_conv(x, w):
    k = w.shape[0]
    return lax.conv_general_dilated(x, w[:, None, :].astype(x.dtype), window_strides=(1,),
                                    padding=[((k - 1) // 2, k // 2)],
                                    dimension_numbers=('NWC', 'WIO', 'NWC'),
                                    feature_group_count=x.shape[-1])


def gated_delta_chunked(q, k, v, g, beta, s0):
    b, n, h = q.shape[:3]
    c = GDN_CHUNK

    def to_chunks(t):
        t = t.astype(F32).reshape((b, n // c, c, h) + t.shape[3:])
        return t.transpose((1, 0, 3, 2) + tuple(range(4, t.ndim)))

    q, k, v, g, beta = to_chunks(q), to_chunks(k), to_chunks(v), to_chunks(g), to_chunks(beta)
    decay = jnp.cumsum(g, axis=-1)
    tril = jnp.tril(jnp.ones((c, c), bool))
    strict = jnp.tril(jnp.ones((c, c), bool), -1)
    diff = decay[..., :, None] - decay[..., None, :]
    gamma = jnp.where(tril, jnp.exp(jnp.where(tril, diff, 0.0)), 0.0)
    kb = k * beta[..., None]
    vb = v * beta[..., None]
    a_mat = jnp.where(strict, jnp.einsum('...id,...jd->...ij', kb, k) * gamma, 0.0)
    eye = jnp.eye(c, dtype=F32)
    rhs = jnp.concatenate([vb, kb * jnp.exp(decay)[..., None]], axis=-1)
    sol = lax.linalg.triangular_solve(a_mat + eye, rhs, left_side=True, lower=True, unit_diagonal=True)
    dv = v.shape[-1]
    u, w = sol[..., :dv], sol[..., dv:]
    qk = jnp.where(tril, jnp.einsum('...id,...jd->...ij', q, k) * gamma, 0.0)
    q_dec = q * jnp.exp(decay)[..., None]
    k_dec = k * jnp.exp(decay[..., -1:] - decay)[..., None]
    last = jnp.exp(decay[..., -1])

    def step(s, inp):
        qd, kd, ww, uu, qkc, lst = inp
        v_new = uu - jnp.einsum('bhck,bhkv->bhcv', ww, s)
        o = jnp.einsum('bhck,bhkv->bhcv', qd, s) + jnp.einsum('bhcj,bhjv->bhcv', qkc, v_new)
        s = s * lst[..., None, None] + jnp.einsum('bhck,bhcv->bhkv', kd, v_new)
        return s, o

    s_fin, o = lax.scan(step, s0.astype(F32), (q_dec, k_dec, w, u, qk, last))
    o = o.transpose(1, 0, 3, 2, 4).reshape(b, n, h, dv)
    return o, s_fin


def gated_deltanet(hc, hl, w_in, conv_w, a_log, dt_bias, o_gain, w_out, need_ctx):
    nqk = GDN_HEADS * GDN_DK
    nv = GDN_HEADS * GDN_DV
    nqkv = 2 * nqk + nv

    def prep(h):
        b, n, _ = h.shape
        p = h @ w_in
        qkv = jax.nn.silu(centred_depthwise_conv(p[..., :nqkv], conv_w))
        q = l2_norm(qkv[..., :nqk].reshape(b, n, GDN_HEADS, GDN_DK)) * (GDN_DK ** -0.5)
        k = l2_norm(qkv[..., nqk:2 * nqk].reshape(b, n, GDN_HEADS, GDN_DK))
        v = qkv[..., 2 * nqk:].reshape(b, n, GDN_HEADS, GDN_DV)
        z = p[..., nqkv:nqkv + nv].reshape(b, n, GDN_HEADS, GDN_DV)
        ab = p[..., nqkv + nv:].astype(F32).reshape(b, n, 2, 2, GDN_HEADS)
        g = -jnp.exp(a_log.astype(F32)) * jax.nn.softplus(ab[:, :, 0] + dt_bias.astype(F32))
        beta = jax.nn.sigmoid(ab[:, :, 1])
        return (q, k, v, g, beta), z

    def scan_dir(t, d, s0):
        q, k, v, g, beta = t
        g, beta = g[:, :, d], beta[:, :, d]
        if d == 1:
            q, k, v, g, beta = [jnp.flip(a, axis=1) for a in (q, k, v, g, beta)]
        o, s = gated_delta_chunked(q, k, v, g, beta, s0)
        if d == 1:
            o = jnp.flip(o, axis=1)
        return o, s

    tc, zc = prep(hc)
    tl, zl = prep(hl)
    s0 = jnp.zeros((hl.shape[0], GDN_HEADS, GDN_DK, GDN_DV), F32)
    oc_f, sc_f = scan_dir(tc, 0, s0)
    ol_f, _ = scan_dir(tl, 0, sc_f)
    oc_b, sc_b = scan_dir(tc, 1, s0)
    ol_b, _ = scan_dir(tl, 1, sc_b)

    def finish(o, z):
        b, n = o.shape[:2]
        o = rms_norm(o.astype(z.dtype), o_gain) * jax.nn.silu(z)
        return o.reshape(b, n, -1) @ w_out

    out_l = finish(ol_f + ol_b, zl)
    out_c = finish(oc_f + oc_b, zc) if need_ctx else None
    return out_c, out_l


def expert_choice_ffn(h, w_router, w_gate, w_up, w_down):
    b, n, _ = h.shape
    cap = EC_FACTOR * n // N_EXPERTS
    aff = jax.nn.softmax((h @ w_router).astype(F32), axis=-1)
    gate, idx = lax.top_k(aff.transpose(0, 2, 1), cap)
    bi = jnp.arange(b)[:, None, None]
    xs = h[bi, idx]
    hg = jnp.einsum('becd,edf->becf', xs, w_gate)
    hu = jnp.einsum('becd,edf->becf', xs, w_up)
    y = jnp.einsum('becf,efd->becd', jax.nn.silu(hg) * hu, w_down) * gate[..., None].astype(h.dtype)
    return jnp.zeros_like(h).at[bi, idx].add(y)


def _count(m):
    return len(range(m, DEPTH, N_MIXERS))


def setup_inputs(seed: int = 0) -> dict:
    key = jax.random.key(seed)
    ks = iter(jax.random.split(key, 48))
    D = D_MODEL
    nA, nB, nC = _count(0), _count(1), _count(2)

    def nrm(shape, s):
        return jax.random.normal(next(ks), shape, F32) * s

    def gain(shape):
        return 1.0 + nrm(shape, 0.02)

    gdn_in = 2 * GDN_HEADS * GDN_DK + 2 * GDN_HEADS * GDN_DV + 4 * GDN_HEADS
    gdn_qkv = 2 * GDN_HEADS * GDN_DK + GDN_HEADS * GDN_DV
    dt = jnp.exp(jax.random.uniform(next(ks), (nC, 2, GDN_HEADS), F32, math.log(1e-3), math.log(1e-1)))
    return {
        'x': nrm((BATCH, SEQ, D), 1.0),
        'c': nrm((BATCH, D), 1.0),
        'ctx': nrm((BATCH, CTX_LEN, D), 1.0),
        'c_ctx': nrm((D,), 1.0),
        'ada_w': nrm((DEPTH, D, 6 * D), 0.5 * D ** -0.5),
        'ada_b': nrm((DEPTH, 6 * D), 0.02),
        'norm_g': gain((DEPTH, 2, D)),
        'da_w_in': nrm((nA, D, 3 * D), D ** -0.5),
        'da_q_gain': gain((nA, DA_HEAD_DIM)),
        'da_k_gain': gain((nA, DA_HEAD_DIM)),
        'da_lambda': nrm((nA, 4, DA_HEAD_DIM), 0.1),
        'da_sub_gain': gain((nA, 2 * DA_HEAD_DIM)),
        'da_w_out': nrm((nA, D, D), D ** -0.5),
        'mla_w_down': nrm((nB, D, MLA_Q_RANK + MLA_KV_RANK + MLA_ROPE), D ** -0.5),
        'mla_q_a_gain': gain((nB, MLA_Q_RANK)),
        'mla_kv_a_gain': gain((nB, MLA_KV_RANK)),
        'mla_w_uq': nrm((nB, MLA_Q_RANK, MLA_HEADS * MLA_QK), MLA_Q_RANK ** -0.5),
        'mla_w_ukv': nrm((nB, MLA_KV_RANK, MLA_HEADS * (MLA_NOPE + MLA_V)), MLA_KV_RANK ** -0.5),
        'mla_q_gain': gain((nB, MLA_QK)),
        'mla_k_gain': gain((nB, MLA_QK)),
        'mla_w_out': nrm((nB, MLA_HEADS * MLA_V, D), (MLA_HEADS * MLA_V) ** -0.5),
        'gdn_w_in': nrm((nC, D, gdn_in), D ** -0.5),
        'gdn_conv_w': nrm((nC, GDN_CONV, gdn_qkv), GDN_CONV ** -0.5),
        'gdn_a_log': jnp.log(jax.random.uniform(next(ks), (nC, 2, GDN_HEADS), F32, 1.0, 16.0)),
        'gdn_dt_bias': dt + jnp.log(-jnp.expm1(-dt)),
        'gdn_o_gain': gain((nC, GDN_DV)),
        'gdn_w_out': nrm((nC, GDN_HEADS * GDN_DV, D), (GDN_HEADS * GDN_DV) ** -0.5),
        'moe_router': nrm((DEPTH, D, N_EXPERTS), D ** -0.5),
        'moe_w_gate': nrm((DEPTH, N_EXPERTS, D, EXPERT_FF), D ** -0.5),
        'moe_w_up': nrm((DEPTH, N_EXPERTS, D, EXPERT_FF), D ** -0.5),
        'moe_w_down': nrm((DEPTH, N_EXPERTS, EXPERT_FF, D), EXPERT_FF ** -0.5),
    }


def reference(x, c, ctx, c_ctx, ada_w, ada_b, norm_g,
              da_w_in, da_q_gain, da_k_gain, da_lambda, da_sub_gain, da_w_out,
              mla_w_down, mla_q_a_gain, mla_kv_a_gain, mla_w_uq, mla_w_ukv, mla_q_gain, mla_k_gain, mla_w_out,
              gdn_w_in, gdn_conv_w, gdn_a_log, gdn_dt_bias, gdn_o_gain, gdn_w_out,
              moe_router, moe_w_gate, moe_w_up, moe_w_down):
    rows = x.shape[1] // GRID_W
    da_cos, da_sin = axial_rope_tables(rows, DA_HEAD_DIM)
    mla_cos, mla_sin = axial_rope_tables(rows, MLA_ROPE)
    cs = ctx
    sc = jax.nn.silu(c)
    scc = jax.nn.silu(c_ctx)
    for l in range(DEPTH):
        last = l == DEPTH - 1
        j = l // N_MIXERS
        kind = l % N_MIXERS
        mod_l = (sc @ ada_w[l] + ada_b[l])[:, None, :]
        mod_c = scc @ ada_w[l] + ada_b[l]
        sh1l, sc1l, g1l, sh2l, sc2l, g2l = jnp.split(mod_l, 6, axis=-1)
        sh1c, sc1c, g1c, sh2c, sc2c, g2c = jnp.split(mod_c, 6, axis=-1)
        hl = rms_norm(x, norm_g[l, 0]) * (1.0 + sc1l) + sh1l
        hc = rms_norm(cs, norm_g[l, 0]) * (1.0 + sc1c) + sh1c
        if kind == 0:
            lam_init = 0.8 - 0.6 * math.exp(-0.3 * l)
            mc, ml = diff_attention(hc, hl, da_w_in[j], da_q_gain[j], da_k_gain[j], da_lambda[j],
                                    da_sub_gain[j], da_w_out[j], lam_init, da_cos, da_sin, not last)
        elif kind == 1:
            mc, ml = mla_attention(hc, hl, mla_w_down[j], mla_q_a_gain[j], mla_kv_a_gain[j], mla_w_uq[j],
                                   mla_w_ukv[j], mla_q_gain[j], mla_k_gain[j], mla_w_out[j],
                                   mla_cos, mla_sin, not last)
        else:
            mc, ml = gated_deltanet(hc, hl, gdn_w_in[j], gdn_conv_w[j], gdn_a_log[j], gdn_dt_bias[j],
                                    gdn_o_gain[j], gdn_w_out[j], not last)
        x = x + g1l * ml
        hl2 = rms_norm(x, norm_g[l, 1]) * (1.0 + sc2l) + sh2l
        x = x + g2l * expert_choice_ffn(hl2, moe_router[l], moe_w_gate[l], moe_w_up[l], moe_w_down[l])
        if not last:
            cs = cs + g1c * mc
            hc2 = rms_norm(cs, norm_g[l, 1]) * (1.0 + sc2c) + sh2c
            cs = cs + g2c * expert_choice_ffn(hc2, moe_router[l], moe_w_gate[l], moe_w_up[l], moe_w_down[l])
    return x
```

```python
import numpy as np
import concourse.bass as bass
import concourse.mybir as mybir
from concourse.bass_utils import run_bass_kernel_spmd

F32 = mybir.dt.float32
BF16 = mybir.dt.bfloat16
I32 = mybir.dt.int32
ALU = mybir.AluOpType
AF = mybir.ActivationFunctionType
AX = mybir.AxisListType

SAME_ENGINE_SYNC = True
SEM_EPOCH = 30000
DMA_EPOCH = 2000


class Ref:
    __slots__ = ("ap", "key")

    def __init__(self, ap, key):
        self.ap = ap
        self.key = key

    def __getitem__(self, idx):
        return Ref(self.ap[idx], self.key)


class TT:
    def __init__(self, handle, key, is_ap=False):
        self.h = handle
        self.key = key

    def __getitem__(self, idx):
        return Ref(self.h[idx], self.key)

    def ref(self, ap):
        return Ref(ap, self.key)


class Prog:
    ENG = ("pe", "act", "dve", "pool", "sp")

    def __init__(self):
        self.nc = bass.Bass("TRN2", target_bir_lowering=False)
        self.ops = []
        self.last_w = {}
        self.readers = {}
        self.ctxs = []
        self.n_t = 0
        self.outs = []
        self.scopes = []
        self.freed = []

    def dram(self, name, shape, dtype, kind):
        t = self.nc.dram_tensor(name, list(shape), dtype, kind=kind)
        return TT(t.ap(), "D:" + name)

    def din(self, name, shape, dtype=F32):
        if not hasattr(self, "_dins"):
            self._dins = {}
        if name not in self._dins:
            self._dins[name] = self.dram(name, shape, dtype, "ExternalInput")
        return self._dins[name]

    def dout(self, name, shape, dtype=F32):
        self.outs.append(name)
        return self.dram(name, shape, dtype, "ExternalOutput")

    def dscratch(self, name, shape, dtype=F32):
        return self.dram(name, shape, dtype, "Internal")

    def tile(self, shape, dtype=F32, name=None):
        self.n_t += 1
        name = (name or "t") + "_%d" % self.n_t
        g = self.nc.sbuf_tensor(name, list(shape), dtype)
        h = g.__enter__()
        (self.scopes[-1] if self.scopes else self.ctxs).append(g)
        return TT(h, "S:" + name)

    def psum(self, shape, dtype=F32, name=None):
        self.n_t += 1
        name = (name or "p") + "_%d" % self.n_t
        g = self.nc.psum_tensor(name, list(shape), dtype)
        h = g.__enter__()
        (self.scopes[-1] if self.scopes else self.ctxs).append(g)
        return TT(h, "P:" + name)

    def push(self):
        self.scopes.append([])

    def pop(self):
        self.barrier()
        for g in reversed(self.scopes.pop()):
            g.__exit__(None, None, None)

    def barrier(self):
        n = len(self.ops)
        if n == 0:
            return
        last = {}
        dl = {}
        for i, o in enumerate(self.ops):
            if o["dma"] is not None:
                dl[o["dma"]] = i
            else:
                last[o["eng"]] = i
        deps = set(last.values()) | set(dl.values())
        for e in self.ENG:
            self.ops.append(dict(eng=e, fn=(lambda en: en.nop()), deps=set(deps), dma=None))
        if hasattr(self, "_pkeys"):
            self._pkeys = {}

    def op(self, eng, fn, reads=(), writes=(), dma_key=None, extra_deps=()):
        i = len(self.ops)
        deps = set(extra_deps)
        rk = [r.key if isinstance(r, Ref) else r for r in reads]
        wk = [w.key if isinstance(w, Ref) else w for w in writes]
        for k in rk:
            if k in self.last_w:
                deps.add(self.last_w[k])
            if isinstance(k, str) and k.startswith("P:"):
                for r in self.readers.get(k, ()):
                    if self.ops[r]["eng"] != eng:
                        deps.add(r)
        for k in wk:
            if k in self.last_w:
                deps.add(self.last_w[k])
            for r in self.readers.get(k, ()):
                deps.add(r)
        deps.discard(i)
        for k in rk:
            self.readers.setdefault(k, []).append(i)
        for k in wk:
            self.last_w[k] = i
            self.readers[k] = []
        if dma_key is not None:
            if not hasattr(self, "_dcount"):
                self._dcount = {}
                self._pkeys = {}
            if dma_key not in self._pkeys:
                self._pkeys[dma_key] = "slot%d" % len(self._pkeys)
            dma_key = self._pkeys[dma_key]
            c = self._dcount.get(dma_key, 0)
            self._dcount[dma_key] = c + 1
            dma_key = (dma_key, c // DMA_EPOCH)
        self.ops.append(dict(eng=eng, fn=fn, deps=deps, dma=dma_key))
        return i

    def mm(self, out, lhsT, rhs, start=True, stop=True, **kw):
        self.op("pe", lambda e: e.matmul(out.ap, lhsT.ap, rhs.ap, start=start, stop=stop, **kw),
                reads=[lhsT, rhs], writes=[out])

    def transpose(self, out, in_, ident):
        self.op("pe", lambda e: e.transpose(out.ap, in_.ap, ident.ap), reads=[in_, ident], writes=[out])

    def act(self, out, in_, func, bias=None, scale=1.0, accum_out=None, eng="act", extra_reads=()):
        reads = [in_] + list(extra_reads)
        kw = {}
        if bias is not None:
            if isinstance(bias, Ref):
                reads.append(bias)
                kw["bias"] = bias.ap
            else:
                kw["bias"] = bias
        if isinstance(scale, Ref):
            reads.append(scale)
            kw["scale"] = scale.ap
        else:
            kw["scale"] = scale
        writes = [out]
        if accum_out is not None:
            writes.append(accum_out)
            kw["accum_out"] = accum_out.ap
        self.op(eng, lambda e: e.activation(out.ap, in_.ap, func, **kw), reads=reads, writes=writes)

    def tt(self, out, in0, in1, op, eng="dve"):
        self.op(eng, lambda e: e.tensor_tensor(out.ap, in0.ap, in1.ap, op), reads=[in0, in1], writes=[out])

    def ts(self, out, in0, s1, s2=None, op0=ALU.mult, op1=None, eng="dve", accum_out=None):
        reads = [in0]
        a1 = s1
        if isinstance(s1, Ref):
            reads.append(s1)
            a1 = s1.ap
        a2 = s2
        if isinstance(s2, Ref):
            reads.append(s2)
            a2 = s2.ap
        writes = [out]
        kw = {}
        if accum_out is not None:
            writes.append(accum_out)
            kw["accum_out"] = accum_out.ap
        if op1 is None:
            self.op(eng, lambda e: e.tensor_scalar(out.ap, in0.ap, a1, None, op0, **kw), reads=reads, writes=writes)
        else:
            self.op(eng, lambda e: e.tensor_scalar(out.ap, in0.ap, a1, a2, op0, op1, **kw), reads=reads, writes=writes)

    def stt(self, out, in0, scalar, in1, op0, op1, eng="dve"):
        reads = [in0, in1]
        a = scalar
        if isinstance(scalar, Ref):
            reads.append(scalar)
            a = scalar.ap
        self.op(eng, lambda e: e.scalar_tensor_tensor(out.ap, in0.ap, a, in1.ap, op0, op1), reads=reads, writes=[out])

    def copy(self, out, in_, eng="dve"):
        if eng == "act":
            self.op(eng, lambda e: e.copy(out.ap, in_.ap), reads=[in_], writes=[out])
        else:
            self.op(eng, lambda e: e.tensor_copy(out.ap, in_.ap), reads=[in_], writes=[out])

    def reduce(self, out, in_, op=ALU.add, axis=AX.X, eng="dve"):
        self.op(eng, lambda e: e.tensor_reduce(out.ap, in_.ap, axis, op), reads=[in_], writes=[out])

    def memset(self, out, val, eng="dve"):
        self.op(eng, lambda e: e.memset(out.ap, val), writes=[out])

    def recip(self, out, in_):
        self.op("dve", lambda e: e.reciprocal(out.ap, in_.ap), reads=[in_], writes=[out])

    def dma(self, out, in_, key="d", q="sp", **kw):
        self.op(q, lambda e: e.dma_start(out=out.ap, in_=in_.ap, **kw), reads=[in_], writes=[out], dma_key=key)

    def build(self):
        nc = self.nc
        ops = self.ops
        n = len(ops)
        signal = [False] * n
        for i, o in enumerate(ops):
            for d in o["deps"]:
                od = ops[d]
                if od["dma"] is None:
                    if od["eng"] == "pe" and o["eng"] == "pe" and o["dma"] is None:
                        continue
                    if (not SAME_ENGINE_SYNC) and od["eng"] == o["eng"] and o["dma"] is None:
                        continue
                    signal[d] = True
        ecnt = {e: 0 for e in self.ENG}
        sigval = [None] * n
        dkeys = {}
        dma_ids = {}
        for i, o in enumerate(ops):
            if o["dma"] is not None:
                dma_ids.setdefault(o["dma"], []).append(i)
            elif signal[i]:
                ecnt[o["eng"]] += 1
                c = ecnt[o["eng"]] - 1
                sigval[i] = (c // SEM_EPOCH, c % SEM_EPOCH + 1)
        import bisect
        stack = []
        esem = {}
        for e in self.ENG:
            for ep in range(max(1, (ecnt[e] + SEM_EPOCH - 1) // SEM_EPOCH)):
                g = nc.semaphore("sem_%s_%d" % (e, ep))
                esem[(e, ep)] = g.__enter__()
                stack.append(g)
        dsem = {}
        for k in dma_ids:
            g = nc.semaphore("dsem_%d" % len(dsem))
            dsem[k] = g.__enter__()
            stack.append(g)
        per_eng = {e: [] for e in self.ENG}
        for i, o in enumerate(ops):
            per_eng[o["eng"]].append(i)
        engobj = {"pe": "tensor", "act": "scalar", "dve": "vector", "pool": "gpsimd", "sp": "sync"}
        self.max_sem = dict(ecnt)

        def emit(ename, e):
            waited = {}
            for i in per_eng[ename]:
                o = ops[i]
                need = {}
                for d in o["deps"]:
                    od = ops[d]
                    if od["dma"] is not None:
                        k = od["dma"]
                        lst = dma_ids[k]
                        cnt = bisect.bisect_left(lst, i)
                        need[("d", k)] = max(need.get(("d", k), 0), 16 * cnt)
                    else:
                        if not signal[d]:
                            continue
                        if od["eng"] == "pe" and ename == "pe" and o["dma"] is None:
                            continue
                        if (not SAME_ENGINE_SYNC) and od["eng"] == ename and o["dma"] is None:
                            continue
                        key = ("e", od["eng"], sigval[d][0])
                        need[key] = max(need.get(key, 0), sigval[d][1])
                for key, v in need.items():
                    if waited.get(key, 0) >= v:
                        continue
                    waited[key] = v
                    s = dsem[key[1]] if key[0] == "d" else esem[(key[1], key[2])]
                    e.wait_ge(s, v)
                ins = o["fn"](e)
                if o["dma"] is not None:
                    ins.then_inc(dsem[o["dma"]], 16)
                elif signal[i]:
                    ins.then_inc(esem[(ename, sigval[i][0])], 1)
            if ename == "sp":
                for k, lst in dma_ids.items():
                    e.wait_ge(dsem[k], 16 * len(lst))

        with nc.Block() as block:
            @block.tensor
            def _(e):
                emit("pe", e)

            @block.scalar
            def _(e):
                emit("act", e)

            @block.vector
            def _(e):
                emit("dve", e)

            @block.gpsimd
            def _(e):
                emit("pool", e)

            @block.sync
            def _(e):
                emit("sp", e)
        for g in reversed(stack):
            g.__exit__(None, None, None)
        for g in reversed(self.ctxs):
            g.__exit__(None, None, None)
        return nc


def run(prog, in_maps, n=8):
    nc = prog.build()
    res = run_bass_kernel_spmd(nc, in_maps, core_ids=list(range(n)))
    return res.results


import math


def R(t, ap):
    return Ref(ap, t.key)


def qblocks(T, CTX):
    out = []
    q = 0
    while q < CTX:
        n = min(512, CTX - q)
        out.append((q, n, CTX // 128))
        q += n
    while q < T:
        n = min(512, T - q)
        out.append((q, n, T // 128))
        q += n
    return out


def attn_core(P, C, QT, KT, V, O, n_maps, dqk, slices_of_map, oslices_of_map, scale, identf):
    T, CTX = C["T"], C["CTX"]
    NT = T // 128
    nsl = len(slices_of_map(0))
    P.push()
    kt = [P.tile([dqk, T], BF16, "kt") for _ in range(2)]
    vt = [P.tile([128, NT, nsl, 65], BF16, "vt") for _ in range(2)]
    qt = [P.tile([dqk, 512], BF16, "qt") for _ in range(2)]
    sp = [P.psum([128, 2, 512], F32, "sp") for _ in range(2)]
    pt = [P.tile([128, 2, 512], BF16, "pt") for _ in range(3)]
    ops = P.psum([65, nsl, 512], F32, "ops")
    osb = [P.tile([65, nsl, 512], F32, "osb") for _ in range(2)]
    tp = P.psum([128, 4, 65], F32, "tp")
    otok = [P.tile([128, 4, 65], F32, "otok") for _ in range(2)]
    blocks = qblocks(T, CTX)
    cnt = 0
    ev = 0
    for m in range(n_maps):
        sl = slices_of_map(m)
        osl = oslices_of_map(m)
        k_ = kt[m % 2]
        v_ = vt[m % 2]
        P.dma(k_[:], KT[m, 0:dqk, :], key="kt%d" % (m % 2))
        P.dma(v_[:], R(V, V.h[:, sl[0]:sl[0] + nsl, :].rearrange("(n p) s e -> p n s e", p=128)), key="vt%d" % (m % 2), q="pool")
        for bi, (q0, nq, nkt) in enumerate(blocks):
            q_ = qt[bi % 2]
            P.dma(q_[:, :nq], QT[m, 0:dqk, q0:q0 + nq], key="qt%d" % (bi % 2))
            iters = [(k2, min(2, nkt - k2)) for k2 in range(0, nkt, 2)]

            def emit_S(ii):
                k2, nk = iters[ii]
                s_ = sp[(cnt + ii) % 2]
                for j in range(nk):
                    P.mm(s_[:, j, :nq], k_[:, (k2 + j) * 128:(k2 + j + 1) * 128], q_[:, :nq])
            emit_S(0)
            for ii, (k2, nk) in enumerate(iters):
                s_ = sp[(cnt + ii) % 2]
                p_ = pt[(cnt + ii) % 3]
                if ii + 1 < len(iters):
                    emit_S(ii + 1)
                P.act(p_[:, :nk, :nq], s_[:, :nk, :nq], AF.Exp, scale=scale)
                for j in range(nk):
                    for s in range(nsl):
                        P.mm(ops[:, s, :nq], v_[:, k2 + j, s, :], p_[:, j, :nq],
                             start=(k2 + j == 0), stop=(k2 + j == nkt - 1))
            cnt += len(iters)
            ob = osb[bi % 2]
            P.copy(ob[:, :, :nq], ops[:, :, :nq], eng="dve")
            for s in range(nsl):
                ot = otok[ev % 2]
                n4 = nq // 128
                for t4 in range(n4):
                    P.transpose(tp[:, t4, :], ob[:, s, t4 * 128:(t4 + 1) * 128], identf[:65, :65])
                P.copy(ot[:, :n4, :], tp[:, :n4, :], eng="act")
                P.dma(R(O, O.h[q0:q0 + nq, osl[s], :].rearrange("(n p) e -> p n e", p=128)), ot[:, :n4, :], key="ot%d" % (ev % 2), q="pool")
                ev += 1
    P.pop()


def attn_core_da(P, C, QT, KT, V2, O2, scale):
    T, CTX = C["T"], C["CTX"]
    NT = T // 128
    P.push()
    kt = [P.tile([64, T], BF16, "kt") for _ in range(2)]
    vt = [P.tile([128, NT, 129], BF16, "vt") for _ in range(2)]
    qt = [P.tile([64, 512], BF16, "qt") for _ in range(2)]
    sp = [P.psum([128, 2, 512], F32, "sp") for _ in range(2)]
    pt = [P.tile([128, 2, 512], BF16, "pt") for _ in range(3)]
    opsT = [P.psum([128, 129], F32, "opsT") for _ in range(4)]
    otok = [P.tile([128, 4, 129], F32, "otok") for _ in range(2)]
    blocks = qblocks(T, CTX)
    cnt = 0
    for m in range(16):
        h = m // 2
        k_ = kt[m % 2]
        v_ = vt[m % 2]
        P.dma(k_[:], KT[m, 0:64, :], key="kt%d" % (m % 2))
        P.dma(v_[:], R(V2, V2.h[:, h, :].rearrange("(n p) e -> p n e", p=128)), key="vt%d" % (m % 2), q="pool")
        for bi, (q0, nq, nkt) in enumerate(blocks):
            q_ = qt[bi % 2]
            P.dma(q_[:, :nq], QT[m, 0:64, q0:q0 + nq], key="qt%d" % (bi % 2))
            n4 = nq // 128
            iters = [(k2, min(2, nkt - k2)) for k2 in range(0, nkt, 2)]

            def emit_S(ii):
                k2, nk = iters[ii]
                s_ = sp[(cnt + ii) % 2]
                for j in range(nk):
                    P.mm(s_[:, j, :nq], k_[:, (k2 + j) * 128:(k2 + j + 1) * 128], q_[:, :nq])
            emit_S(0)
            for ii, (k2, nk) in enumerate(iters):
                s_ = sp[(cnt + ii) % 2]
                p_ = pt[(cnt + ii) % 3]
                if ii + 1 < len(iters):
                    emit_S(ii + 1)
                P.act(p_[:, :nk, :nq], s_[:, :nk, :nq], AF.Exp, scale=scale)
                for j in range(nk):
                    kti = k2 + j
                    for t4 in range(n4):
                        P.mm(opsT[t4][:, :], p_[:, j, t4 * 128:(t4 + 1) * 128], v_[:, kti, :],
                             start=(kti == 0), stop=(kti == nkt - 1))
            cnt += len(iters)
            ot = otok[bi % 2]
            for t4 in range(n4):
                P.copy(ot[:, t4, :], opsT[t4][:, :], eng="dve")
            P.dma(R(O2, O2.h[q0:q0 + nq, m, :].rearrange("(n p) e -> p n e", p=128)), ot[:, :n4, :], key="ot%d" % (bi % 2), q="pool")
    P.pop()


EPS = 1e-6


def dview(t, ap):
    return Ref(ap, t.key)


class Net:
    def __init__(self, P, C):
        self.P = P
        self.C = C
        T = C["T"]
        self.NT = T // 128
        self.nctx = C["CTX"] // 128
        self.identf = P.tile([128, 128], F32, "identf")
        self.identb = P.tile([128, 128], BF16, "identb")
        idd = P.din("ident", [128, 128])
        P.dma(self.identf[:], idd[:], key="c")
        P.copy(self.identb[:], self.identf[:])
        self.X = P.dscratch("X", [T, 1024], F32)
        self.H2 = P.dscratch("H2", [T, 1024], BF16)
        self.AFF = P.dscratch("AFF", [T, 16], F32)
        self.MOD = P.dscratch("MOD", [2, 6, 128, 1024], F32)
        self.MOE = P.dscratch("MOE", [T, 1024], F32)
        self.QT = P.dscratch("QT", [16, 96, T], BF16)
        self.KT = P.dscratch("KT", [16, 96, T], BF16)
        self.V = P.dscratch("V", [T, 16, 65], BF16)
        self.O = P.dscratch("O", [T, 32, 65], F32)
        self.V2 = P.dscratch("V2", [T, 8, 129], BF16)
        self.O2 = P.dscratch("O2", [T, 16, 129], F32)
        self.AFFT = P.dscratch("AFFT", [16, T], F32)
        self.xin = P.din("xall", [T, 1024])
        self.cT = P.din("cT", [128, 8, 2])
        self.out = P.dout("out", [C["SEQ"], 1024])
        self.epsb = P.tile([128, 1], F32, "epsb")
        self.POSF = P.tile([128, self.NT, 16], F32, "POSF")
        self.RH = P.tile([128, self.NT, 16, 5], BF16, "RH")
        self.idxt = [P.tile([128, 16], I32, "idx") for _ in range(2)]
        self.xst = [P.tile([128, 1024], BF16, "xs") for _ in range(2)]
        self.ygt = [P.tile([128, 1024], F32, "yg") for _ in range(2)]
        P.memset(self.epsb[:], EPS)

    def rstd(self, out, ss, n):
        P = self.P
        P.ts(out, ss, 1.0 / n, EPS, ALU.mult, ALU.add)
        P.act(out, out, AF.Sqrt)
        P.recip(out, out)

    def load_w(self, dst, src_ap, nk, N, stg, cnt, q="sp", key="w"):
        P = self.P
        for k in range(nk):
            P.dma(dst[:, k, :], Ref(src_ap[k * 128:(k + 1) * 128, :], "W"), key="%s%d" % (key, k % 2), q="pool")

    def transposeT(self, dstT, src, nblk, pT, eng="act"):
        P = self.P
        for b0 in range(0, nblk, 8):
            nb = min(8, nblk - b0)
            for j in range(nb):
                P.transpose(pT[:, j, :], src[:, (b0 + j) * 128:(b0 + j + 1) * 128], self.identb[:])
            P.copy(dstT[:, b0:b0 + nb, :], pT[:, :nb, :], eng=eng)

    def front(self, xt, A, B, sq, small, tmp, hb):
        P = self.P
        ss, rs = small
        P.act(sq[:], xt[:], AF.Square, accum_out=ss[:])
        self.rstd(rs[:], ss[:], 1024)
        P.stt(tmp[:], xt[:], rs[:], A, ALU.mult, ALU.mult)
        P.tt(hb[:], tmp[:], B, ALU.add, eng="pool")

    def mod_phase(self, l):
        P, C = self.P, self.C
        P.push()
        adaw = P.din("ada_w%d" % l, [1024, 6144])
        adab = P.din("ada_b%d" % l, [6144])
        ng = P.din("ng%d" % l, [2, 1024])
        ct = P.tile([128, 8, 2], F32, "ct")
        P.dma(ct[:], self.cT[:], key="m0")
        st = P.tile([128, 8, 2], F32, "st")
        P.act(st[:], ct[:], AF.Silu)
        sbc = P.tile([128, 8, 2, 128], F32, "sbc")
        P.copy(sbc[:], R(st, st.h[:].unsqueeze(3).broadcast_to([128, 8, 2, 128])))
        wb = [P.tile([128, 8, 512], F32, "wb") for _ in range(2)]
        bb = [P.tile([128, 512], F32, "bb") for _ in range(2)]
        gb = [P.tile([128, 512], F32, "gb") for _ in range(2)]
        pm = [P.psum([128, 512], F32, "pm") for _ in range(2)]
        res = [P.tile([128, 512], F32, "res") for _ in range(2)]
        slot_of = {0: 1, 1: 0, 2: 2, 3: 4, 4: 3, 5: 5}
        it = 0
        for blk in range(12):
            chunk = blk // 2
            c0 = (blk % 2) * 512
            w_ = wb[blk % 2]
            P.dma(w_[:], R(adaw, adaw.h[:, blk * 512:(blk + 1) * 512].rearrange("(k p) n -> p k n", p=128)), key="mw%d" % (blk % 2))
            b_ = bb[blk % 2]
            P.dma(b_[:], R(adab, adab.h[blk * 512:(blk + 1) * 512].partition_broadcast(128)), key="mb%d" % (blk % 2), q="pool")
            g_ = gb[blk % 2]
            if chunk in (1, 4):
                P.dma(g_[:], R(ng, ng.h[0 if chunk == 1 else 1, c0:c0 + 512].partition_broadcast(128)), key="mg%d" % (blk % 2), q="pool")
            for which in range(2):
                p_ = pm[it % 2]
                r_ = res[it % 2]
                it += 1
                cj = 1 if which == 0 else 0
                for k in range(8):
                    P.mm(p_[:], sbc[:, k, cj, :], w_[:, k, :], start=(k == 0), stop=(k == 7))
                P.tt(r_[:], p_[:], b_[:], ALU.add)
                if chunk in (1, 4):
                    P.stt(r_[:], r_[:], 1.0, g_[:], ALU.add, ALU.mult)
                P.dma(self.MOD[which, slot_of[chunk], :, c0:c0 + 512], r_[:], key="mo%d" % (it % 2), q="pool")
        P.pop()

    def load_mod(self, slots):
        P = self.P
        d = {}
        for which in range(2):
            for s in slots:
                t = P.tile([128, 1024], F32, "mod")
                P.dma(t[:], self.MOD[which, s], key="lm", q="pool")
                d[(which, s)] = t
        return d

    def xsrc(self, l):
        import os
        return self.xin if l == int(os.environ.get("MK_L0", "0")) else self.X

    def da_proj(self, l, j):
        P, C = self.P, self.C
        T, NT = C["T"], self.NT
        P.push()
        win = P.din("da_win%d" % j, [1024, 3072])
        qg = P.din("da_qg%d" % j, [64])
        kg = P.din("da_kg%d" % j, [64])
        cosd = P.din("cosD", [T, 32]) if "cosD" not in self.__dict__ else self.cosD
        self.cosD = cosd
        sind = P.din("sinD", [T, 32]) if "sinD" not in self.__dict__ else self.sinD
        self.sinD = sind
        w = P.tile([128, 8, 3072], BF16, "win")
        stg = [P.tile([128, 2048], F32, "stg") for _ in range(2)]
        self.load_w(w, win.h, 8, 3072, stg, [0])
        md = self.load_mod([0, 1])
        qkg = P.tile([128, 32, 64], F32, "qkg")
        P.dma(qkg[:, 0:16, :], R(qg, qg.h[:].partition_broadcast(128).unsqueeze(1).broadcast_to([128, 16, 64])), key="g0", q="pool")
        P.dma(qkg[:, 16:32, :], R(kg, kg.h[:].partition_broadcast(128).unsqueeze(1).broadcast_to([128, 16, 64])), key="g0", q="pool")
        xt = [P.tile([128, 1024], F32, "xt") for _ in range(2)]
        cs = [P.tile([128, 2, 32], F32, "cs") for _ in range(2)]
        sq = P.tile([128, 2048], F32, "sq")
        tmp = P.tile([128, 1024], F32, "tmp")
        hb = P.tile([128, 1024], BF16, "hb")
        hT = [P.tile([128, 8, 128], BF16, "hT") for _ in range(2)]
        small = [(P.tile([128, 1], F32, "ss"), P.tile([128, 1], F32, "rs")) for _ in range(2)]
        ss32 = P.tile([128, 32], F32, "ss32")
        rs32 = P.tile([128, 32], F32, "rs32")
        qn = P.tile([128, 32, 64], F32, "qn")
        t1 = P.tile([128, 32, 32], F32, "t1")
        t2 = P.tile([128, 32, 32], F32, "t2")
        t3 = P.tile([128, 32, 32], F32, "t3")
        t4 = P.tile([128, 32, 32], F32, "t4")
        qb = P.tile([128, 32, 64], BF16, "qb")
        qkT = [P.tile([128, 16, 128], BF16, "qkT") for _ in range(2)]
        va = [P.tile([128, 8, 129], BF16, "va") for _ in range(2)]
        for v_ in va:
            P.memset(v_[:], 1.0)
        pT = [P.psum([128, 8, 128], BF16, "pT") for _ in range(2)]
        pqk = P.psum([128, 4, 512], F32, "pqk")
        pv = P.psum([128, 2, 512], F32, "pv")
        X = self.xsrc(l)
        for i in range(NT):
            which = 0 if i < self.nctx else 1
            x_ = xt[i % 2]
            P.dma(x_[:], X[i * 128:(i + 1) * 128, :], key="x%d" % (i % 2))
            c_ = cs[i % 2]
            P.dma(c_[:, 0, :], cosd[i * 128:(i + 1) * 128, :], key="cs%d" % (i % 2), q="pool")
            P.dma(c_[:, 1, :], sind[i * 128:(i + 1) * 128, :], key="cs%d" % (i % 2), q="pool")
            self.front(x_, md[(which, 0)][:], md[(which, 1)][:], R(sq, sq.h[:, :1024]), small[i % 2], tmp, hb)
            h_ = hT[i % 2]
            self.transposeT(h_, hb, 8, pT[0])
            for n in range(4):
                for k in range(8):
                    P.mm(pqk[:, n, :], h_[:, k, :], w[:, k, n * 512:(n + 1) * 512], start=(k == 0), stop=(k == 7))
            for n in range(2):
                for k in range(8):
                    P.mm(pv[:, n, :], h_[:, k, :], w[:, k, 2048 + n * 512:2048 + (n + 1) * 512], start=(k == 0), stop=(k == 7))
            pq3 = R(pqk, pqk.h[:].rearrange("p n (h d) -> p (n h) d", d=64))
            P.act(sq[:], R(pqk, pqk.h[:].rearrange("p n f -> p (n f)")), AF.Square)
            P.reduce(ss32[:], R(sq, sq.h[:].rearrange("p (h d) -> p h d", d=64)))
            self.rstd(rs32[:], ss32[:], 64)
            P.tt(qn[:], pq3, R(rs32, rs32.h[:].unsqueeze(2).broadcast_to([128, 32, 64])), ALU.mult)
            P.tt(qn[:], qn[:], qkg[:], ALU.mult, eng="pool")
            cosb = R(c_, c_.h[:, 0, :].unsqueeze(1).broadcast_to([128, 32, 32]))
            sinb = R(c_, c_.h[:, 1, :].unsqueeze(1).broadcast_to([128, 32, 32]))
            x1 = qn[:, :, 0:32]
            x2 = qn[:, :, 32:64]
            P.tt(t1[:], x1, cosb, ALU.mult)
            P.tt(t2[:], x2, sinb, ALU.mult, eng="pool")
            P.tt(qb[:, :, 0:32], t1[:], t2[:], ALU.subtract)
            P.tt(t3[:], x2, cosb, ALU.mult, eng="pool")
            P.tt(t4[:], x1, sinb, ALU.mult)
            P.tt(qb[:, :, 32:64], t3[:], t4[:], ALU.add, eng="pool")
            q_ = qkT[i % 2]
            self.transposeT(q_, R(qb, qb.h[:].rearrange("p h d -> p (h d)")), 16, pT[1], eng="dve")
            for two in range(2):
                P.dma(R(self.QT, self.QT.h[:, 0:64, i * 128:(i + 1) * 128].rearrange("(j two) d t -> two d j t", two=2)[two]), q_[two * 64:(two + 1) * 64, 0:8, :], key="qo%d" % (i % 2))
                P.dma(R(self.KT, self.KT.h[:, 0:64, i * 128:(i + 1) * 128].rearrange("(j two) d t -> two d j t", two=2)[two]), q_[two * 64:(two + 1) * 64, 8:16, :], key="qo%d" % (i % 2))
            v_ = va[i % 2]
            P.copy(v_[:, :, 0:128], R(pv, pv.h[:].rearrange("p n (h d) -> p (n h) d", d=128)), eng="act")
            P.dma(self.V2[i * 128:(i + 1) * 128, :, :], v_[:], key="vo%d" % (i % 2), q="pool")
        P.pop()

    def post_alloc(self, l, wout_name):
        P = self.P
        wo_d = P.din(wout_name, [1024, 1024])
        rt_d = P.din("rt%d" % l, [1024, 16])
        S = {}
        S["wo"] = P.tile([128, 8, 1024], BF16, "wo")
        stg = [P.tile([128, 2048], F32, "stg") for _ in range(2)]
        self.load_w(S["wo"], wo_d.h, 8, 1024, stg, [0])
        rtf = P.tile([128, 8, 16], F32, "rtf")
        P.dma(rtf[:], R(rt_d, rt_d.h.rearrange("(k p) n -> p k n", p=128)), key="rt")
        S["rt"] = P.tile([128, 8, 16], BF16, "rtb")
        P.copy(S["rt"][:], rtf[:])
        S["md"] = self.load_mod([2, 3, 4])
        S["oT"] = [P.tile([128, 8, 128], BF16, "oT") for _ in range(2)]
        S["xt"] = [P.tile([128, 1024], F32, "xt") for _ in range(2)]
        S["xn"] = [P.tile([128, 1024], F32, "xn") for _ in range(2)]
        S["sq"] = P.tile([128, 1024], F32, "sq")
        S["tmp"] = P.tile([128, 1024], F32, "tmp")
        S["h2"] = [P.tile([128, 1024], BF16, "h2") for _ in range(2)]
        S["h2T"] = [P.tile([128, 8, 128], BF16, "h2T") for _ in range(2)]
        S["small"] = [(P.tile([128, 1], F32, "ss"), P.tile([128, 1], F32, "rs")) for _ in range(2)]
        S["sm"] = [[P.tile([128, 1], F32, "sm") for _ in range(3)] for _ in range(2)]
        S["e"] = [P.tile([128, 16], F32, "e") for _ in range(2)]
        S["aff"] = [P.tile([128, 16], F32, "aff") for _ in range(2)]
        S["pT"] = P.psum([128, 8, 128], BF16, "pT")
        S["py"] = P.psum([128, 2, 512], F32, "py")
        S["pr"] = P.psum([128, 16], F32, "pr")
        S["pa"] = P.psum([16, 128], F32, "pa")
        S["at"] = [P.tile([16, 128], F32, "at") for _ in range(2)]
        return S

    def post(self, l, S, i, ob):
        P = self.P
        which = 0 if i < self.nctx else 1
        md = S["md"]
        oT = S["oT"][i % 2]
        self.transposeT(oT, ob, 8, S["pT"])
        py = S["py"]
        for n in range(2):
            for k in range(8):
                P.mm(py[:, n, :], oT[:, k, :], S["wo"][:, k, n * 512:(n + 1) * 512], start=(k == 0), stop=(k == 7))
        x_ = S["xt"][i % 2]
        X = self.xsrc(l)
        P.dma(x_[:], X[i * 128:(i + 1) * 128, :], key="px%d" % (i % 2))
        xn = S["xn"][i % 2]
        P.tt(xn[:], R(py, py.h[:].rearrange("p n f -> p (n f)")), md[(which, 2)][:], ALU.mult)
        P.tt(xn[:], xn[:], x_[:], ALU.add, eng="pool")
        P.dma(self.X[i * 128:(i + 1) * 128, :], xn[:], key="pxo%d" % (i % 2), q="pool")
        h2 = S["h2"][i % 2]
        self.front(xn, md[(which, 3)][:], md[(which, 4)][:], S["sq"], S["small"][i % 2], S["tmp"], h2)
        P.dma(self.H2[i * 128:(i + 1) * 128, :], h2[:], key="ph%d" % (i % 2), q="pool")
        h2T = S["h2T"][i % 2]
        self.transposeT(h2T, h2, 8, S["pT"])
        pr = S["pr"]
        for k in range(8):
            P.mm(pr[:], h2T[:, k, :], S["rt"][:, k, :], start=(k == 0), stop=(k == 7))
        mx, sm, rc = S["sm"][i % 2]
        P.reduce(mx[:], pr[:], op=ALU.max)
        P.ts(mx[:], mx[:], -1.0, None, ALU.mult)
        e = S["e"][i % 2]
        P.act(e[:], pr[:], AF.Exp, bias=mx[:], scale=1.0, accum_out=sm[:])
        P.recip(rc[:], sm[:])
        aff = S["aff"][i % 2]
        P.ts(aff[:], e[:], rc[:], None, ALU.mult)
        P.dma(self.AFF[i * 128:(i + 1) * 128, :], aff[:], key="pa%d" % (i % 2), q="pool")
        P.transpose(S["pa"][:], aff[:], self.identf[:])
        at = S["at"][i % 2]
        P.copy(at[:], S["pa"][:], eng="act")
        P.dma(self.AFFT[:, i * 128:(i + 1) * 128], at[:], key="pat%d" % (i % 2), q="pool")

    def da_finish(self, l, j):
        P, C = self.P, self.C
        NT = self.NT
        lam_init = 0.8 - 0.6 * math.exp(-0.3 * l)
        P.push()
        S = self.post_alloc(l, "da_wout%d" % j)
        lamd = P.din("da_lam%d" % j, [256])
        sgd = P.din("da_sg%d" % j, [128])
        lt = P.tile([128, 256], F32, "lt")
        P.dma(lt[:], R(lamd, lamd.h[:].partition_broadcast(128)), key="fl")
        pr2 = P.tile([128, 2, 64], F32, "pr2")
        P.tt(pr2[:, 0, :], lt[:, 0:64], lt[:, 64:128], ALU.mult)
        P.tt(pr2[:, 1, :], lt[:, 128:192], lt[:, 192:256], ALU.mult)
        s2 = P.tile([128, 2], F32, "s2")
        P.reduce(s2[:], pr2[:])
        e2 = P.tile([128, 2], F32, "e2")
        P.act(e2[:], s2[:], AF.Exp)
        nl = P.tile([128, 1], F32, "nl")
        P.tt(nl[:], e2[:, 1:2], e2[:, 0:1], ALU.subtract)
        P.ts(nl[:], nl[:], -lam_init, None, ALU.add)
        sg = P.tile([128, 8, 128], F32, "sg")
        P.dma(sg[:], R(sgd, sgd.h[:].partition_broadcast(128).unsqueeze(1).broadcast_to([128, 8, 128])), key="fl")
        P.ts(sg[:], sg[:], 1.0 - lam_init, None, ALU.mult)
        ot = [P.tile([128, 16, 129], F32, "ot") for _ in range(2)]
        rd = P.tile([128, 16], F32, "rd")
        on = P.tile([128, 16, 128], F32, "on")
        df = P.tile([128, 8, 128], F32, "df")
        sq = P.tile([128, 8, 128], F32, "sqd")
        ss8 = P.tile([128, 8], F32, "ss8")
        rs8 = P.tile([128, 8], F32, "rs8")
        ob = [P.tile([128, 1024], BF16, "ob") for _ in range(2)]
        for i in range(NT):
            o_ = ot[i % 2]
            P.dma(o_[:], self.O2[i * 128:(i + 1) * 128, :, :], key="fo%d" % (i % 2))
            P.recip(rd[:], o_[:, :, 128])
            P.tt(on[:], o_[:, :, 0:128], R(rd, rd.h[:].unsqueeze(2).broadcast_to([128, 16, 128])), ALU.mult)
            on4 = on.h[:].rearrange("p (h m) d -> p h m d", m=2)
            P.stt(df[:], R(on, on4[:, :, 1, :]), nl[:], R(on, on4[:, :, 0, :]), ALU.mult, ALU.add)
            P.act(sq[:], df[:], AF.Square)
            P.reduce(ss8[:], sq[:])
            self.rstd(rs8[:], ss8[:], 128)
            P.tt(df[:], df[:], R(rs8, rs8.h[:].unsqueeze(2).broadcast_to([128, 8, 128])), ALU.mult)
            o2 = ob[i % 2]
            P.tt(R(o2, o2.h[:].rearrange("p (h d) -> p h d", d=128)), df[:], sg[:], ALU.mult, eng="pool")
            self.post(l, S, i, o2)
        P.pop()

    def moe_phase(self, l, last):
        P, C = self.P, self.C
        T, NT, CTX, SEQ, FF, NE = C["T"], self.NT, C["CTX"], C["SEQ"], C["FF"], 16
        nctx = self.nctx
        NF = FF // 128
        sets = [(0, CTX, 2 * CTX // NE), (CTX, SEQ, 2 * SEQ // NE)]
        nrows = sum(NE * s_[2] for s_ in sets)
        POSF, RH = self.POSF, self.RH
        P.push()
        zt = P.tile([128, 1024], F32, "zt")
        P.memset(zt[:], 0.0)
        for i in range(NT):
            P.dma(self.MOE[i * 128:(i + 1) * 128, :], zt[:], key="mz", q="pool")
        maskT = P.tile([16, T], BF16, "maskT")
        lo = P.tile([16, 1], F32, "lo")
        hi = P.tile([16, 1], F32, "hi")
        mid = P.tile([16, 1], F32, "mid")
        cntt = P.tile([16, 1], F32, "cnt")
        mm_ = P.tile([16, 1], F32, "m")
        d1 = P.tile([16, 1], F32, "d1")
        d2 = P.tile([16, 1], F32, "d2")
        cmp = P.tile([16, max(CTX, SEQ)], F32, "cmp")
        affT = P.tile([16, T], F32, "affT")
        P.dma(affT[:], self.AFFT[:], key="mza")
        for (t0, n, cap) in sets:
            a_ = affT[:, t0:t0 + n]
            P.memset(lo[:], 0.0)
            P.memset(hi[:], 1.0001)
            for it in range(34):
                P.tt(mid[:], lo[:], hi[:], ALU.add)
                P.ts(mid[:], mid[:], 0.5, None, ALU.mult)
                P.ts(cmp[:, :n], a_, mid[:], None, ALU.is_ge)
                P.reduce(cntt[:], cmp[:, :n])
                P.ts(mm_[:], cntt[:], float(cap) - 0.5, None, ALU.is_ge)
                P.tt(d1[:], mid[:], lo[:], ALU.subtract)
                P.tt(d2[:], hi[:], mid[:], ALU.subtract)
                P.stt(lo[:], d1[:], mm_[:], lo[:], ALU.mult, ALU.add)
                P.stt(hi[:], d2[:], mm_[:], mid[:], ALU.mult, ALU.add)
            P.ts(maskT[:, t0:t0 + n], a_, lo[:], None, ALU.is_ge)
        U = P.tile([128, 128], BF16, "U")
        ones = P.tile([128, 128], BF16, "ones")
        Ud = P.din("U", [128, 128])
        uf = P.tile([128, 128], F32, "uf")
        P.dma(uf[:], Ud[:], key="cu")
        P.copy(U[:], uf[:])
        P.memset(ones[:], 1.0)
        tokd = P.din("tokidx", [128, NT])
        tokt = P.tile([128, NT], F32, "tokt")
        P.dma(tokt[:], tokd[:], key="cu")
        ecap = P.tile([128, 2, 16], F32, "ecap")
        ecd = P.din("ecap", [128, 2, 16])
        P.dma(ecap[:], ecd[:], key="cu")
        dumpd = P.din("dump", [128, 1])
        dump = P.tile([128, 1], F32, "dump")
        P.dma(dump[:], dumpd[:], key="cu")
        cum = P.tile([128, 16], BF16, "cum")
        cumf = P.tile([128, 16], F32, "cumf")
        pm = P.psum([128, 16], BF16, "pmk")
        pp = P.psum([128, 16], F32, "ppos")
        mt = [P.tile([128, 16], BF16, "mt") for _ in range(2)]
        mtf = [P.tile([128, 16], F32, "mtf") for _ in range(2)]
        pos = [P.tile([128, 16], F32, "pos") for _ in range(2)]
        ok = [P.tile([128, 16], F32, "ok") for _ in range(2)]
        afft = [P.tile([128, 16], F32, "afft") for _ in range(2)]
        gr1 = P.tile([128, 16], F32, "gr1")
        gr2 = P.tile([128, 16], F32, "gr2")
        ipcd = P.din("ipc", [128, NT, 2])
        ipc = P.tile([128, NT, 2], F32, "ipc")
        P.dma(ipc[:], ipcd[:], key="cu")
        for si, (t0, n, cap) in enumerate(sets):
            P.memset(cumf[:], 0.0)
            P.copy(cum[:], cumf[:])
            for i in range(t0 // 128, (t0 + n) // 128):
                m_ = mt[i % 2]
                P.transpose(pm[:], maskT[:, i * 128:(i + 1) * 128], self.identb[:16, :16])
                P.copy(m_[:], pm[:])
                P.copy(mtf[i % 2][:], pm[:], eng="act")
                P.mm(pp[:], U[:], m_[:], start=True, stop=False)
                P.mm(pp[:], ones[:], cum[:], start=False, stop=True)
                p_ = pos[i % 2]
                P.copy(p_[:], pp[:])
                P.tt(cumf[:], cumf[:], mtf[i % 2][:], ALU.add)
                P.copy(cum[:], cumf[:])
                o_ = ok[i % 2]
                P.ts(o_[:], p_[:], float(cap) - 0.5, None, ALU.is_lt)
                P.tt(o_[:], o_[:], mtf[i % 2][:], ALU.mult)
                P.ts(p_[:], p_[:], 1.0, None, ALU.add)
                P.tt(p_[:], p_[:], o_[:], ALU.mult)
                P.ts(POSF[:, i, :], p_[:], -1.0, None, ALU.add)
                a_ = afft[i % 2]
                P.dma(a_[:], self.AFF[i * 128:(i + 1) * 128, :], key="ca%d" % (i % 2))
                P.copy(RH[:, i, :, 0:2], R(ipc, ipc.h[:, i, :].unsqueeze(1).broadcast_to([128, 16, 2])), eng="pool")
                P.copy(RH[:, i, :, 2], a_[:])
                P.tt(gr1[:], a_[:], RH[:, i, :, 2], ALU.subtract)
                P.copy(RH[:, i, :, 3], gr1[:])
                P.tt(gr2[:], gr1[:], RH[:, i, :, 3], ALU.subtract)
                P.copy(RH[:, i, :, 4], gr2[:])
        P.pop()

        P.push()
        wgd = P.din("wg%d" % l, [NE, 1024, FF])
        wud = P.din("wu%d" % l, [NE, 1024, FF])
        wdd = P.din("wd%d" % l, [NE, FF, 1024])
        GW = min(512, FF)
        NG = FF // GW
        F4 = GW // 128
        wgg = [P.tile([128, 8, GW], BF16, "wgg") for _ in range(2)]
        wug = [P.tile([128, 8, GW], BF16, "wug") for _ in range(2)]
        wd = P.tile([128, NF, 1024], BF16, "wd")
        cnt = [0]
        slot_tiles = []
        s0 = 0
        for si, (t0, n, cap) in enumerate(sets):
            for b0 in range(0, cap, 128):
                ns = min(128, cap - b0)
                slot_tiles.append((si, b0, s0, ns))
                s0 += ns
        NSLOT = s0
        NTS = len(slot_tiles)
        blocks = []
        b = 0
        for si, (t0, n, cap) in enumerate(sets):
            for b0 in range(0, cap, 512):
                nb = min(512, cap - b0)
                blocks.append((b, nb))
                b += nb
        xs = self.xst
        xsT = P.tile([128, 8, NSLOT], BF16, "xsT")
        actT = P.tile([128, NF, NSLOT], BF16, "actT")
        lst = [P.tile([128, NTS, 2], F32, "lst") for _ in range(2)]
        idx = self.idxt
        sg_ = [P.tile([128, 512], F32, "sgl") for _ in range(2)]
        yg = self.ygt
        pT = P.psum([128, 8, 128], BF16, "pT")
        pg = [P.psum([128, 512], F32, "pg") for _ in range(2)]
        pu = [P.psum([128, 512], F32, "pu") for _ in range(2)]
        py = P.psum([128, 2, 512], F32, "py")
        bases = []
        base = 0
        for (t0, n, cap) in sets:
            bases.append(base)
            base += NE * cap

        def stage_cast(dst_ref, src_ap, key):
            P.dma(dst_ref, Ref(src_ap, "W"), key=key, q="pool")

        def load_group(e, g):
            gi = e * NG + g
            for (dst, srcd) in ((wgg[gi % 2], wgd), (wug[gi % 2], wud)):
                src3 = srcd.h[e].rearrange("(k p) n -> p k n", p=128)
                for k0 in range(0, 8, 4):
                    stage_cast(dst[:, k0:k0 + 4, :], src3[:, k0:k0 + 4, g * GW:(g + 1) * GW], "wq%d" % (gi % 2))

        def load_wd(e):
            src3 = wdd.h[e].rearrange("(f p) n -> p f n", p=128)
            for f0 in range(0, NF, 2):
                stage_cast(wd[:, f0:f0 + 2, :], src3[:, f0:f0 + 2, :], "wdk")
        iotad = P.din("iota", [128, 1024])
        iota = P.tile([128, 1024], F32, "iota")
        P.dma(iota[:], iotad[:], key="cu")
        oh = [P.tile([128, 128], BF16, "oh") for _ in range(4)]
        pacc2 = P.psum([128, 2, 8], F32, "pacc")
        ohc = [0]
        pacs = [P.tile([128, 5], F32, "pacs") for _ in range(2)]

        def compact(e):
            l_ = lst[e % 2]
            P.memset(l_[:], 0.0)
            for ti, (si, b0, sl0, ns) in enumerate(slot_tiles):
                t0, n, cap = sets[si]
                tiles = list(range(t0 // 128, (t0 + n) // 128))
                pa = pacc2[:, ti % 2, :]
                for i in tiles:
                    o_ = oh[ohc[0] % 4]
                    ohc[0] += 1
                    P.ts(o_[:, :ns], iota[:, b0:b0 + ns], POSF[:, i, e:e + 1], None, ALU.is_equal)
                    P.mm(pa[:ns, 0:5], o_[:, :ns], RH[:, i, e, :], start=(i == tiles[0]), stop=(i == tiles[-1]))
                pc = pacs[ti % 2]
                P.copy(pc[:ns, :], pa[:ns, 0:5], eng="act")
                P.stt(l_[:ns, ti, 0:1], pc[:ns, 0:1], 128.0, pc[:ns, 1:2], ALU.mult, ALU.add)
                P.reduce(l_[:ns, ti, 1:2], pc[:ns, 2:5])
        compact(0)
        load_group(0, 0)
        for e in range(NE):
            l_ = lst[e % 2]
            i_ = idx[e % 2]
            P.copy(i_[:, :NTS], l_[:, :, 0])
            for ti, (si, b0, sl0, ns) in enumerate(slot_tiles):
                x_ = xs[ti % 2]
                a1, a2, a3 = x_.h[:ns, :], self.H2.h[:, :], i_.h[:ns, ti:ti + 1]
                P.op("pool", (lambda en, a1=a1, a2=a2, a3=a3: en.indirect_dma_start(
                    out=a1, out_offset=None, in_=a2, in_offset=bass.IndirectOffsetOnAxis(ap=a3, axis=0))),
                    reads=[self.H2[:], i_[:]], writes=[x_[:]], dma_key="fg%d" % (ti % 2))
                for k in range(8):
                    P.transpose(pT[:, k, :ns], x_[:ns, k * 128:(k + 1) * 128], self.identb[:ns, :ns])
                P.copy(xsT[:, :, sl0:sl0 + ns], pT[:, :, :ns], eng="act")
            if e + 1 < NE:
                compact(e + 1)
            for g in range(NG):
                if g + 1 < NG:
                    load_group(e, g + 1)
                elif e + 1 < NE:
                    load_group(e + 1, 0)
                if g == 0:
                    load_wd(e)
                wg_, wu_ = wgg[(e * NG + g) % 2], wug[(e * NG + g) % 2]
                for (bs, nb) in blocks:
                    for f4 in range(F4):
                        f = g * F4 + f4
                        g_ = pg[f % 2]
                        u_ = pu[f % 2]
                        for k in range(8):
                            P.mm(g_[:, :nb], wg_[:, k, f4 * 128:(f4 + 1) * 128], xsT[:, k, bs:bs + nb], start=(k == 0), stop=(k == 7))
                        for k in range(8):
                            P.mm(u_[:, :nb], wu_[:, k, f4 * 128:(f4 + 1) * 128], xsT[:, k, bs:bs + nb], start=(k == 0), stop=(k == 7))
                        s_ = sg_[f % 2]
                        P.act(s_[:, :nb], g_[:, :nb], AF.Silu)
                        P.tt(actT[:, f, bs:bs + nb], s_[:, :nb], u_[:, :nb], ALU.mult)
            for ti, (si, b0, sl0, ns) in enumerate(slot_tiles):
                for nn in range(2):
                    for f in range(NF):
                        P.mm(py[:ns, nn, :], actT[:, f, sl0:sl0 + ns], wd[:, f, nn * 512:(nn + 1) * 512], start=(f == 0), stop=(f == NF - 1))
                y_ = yg[ti % 2]
                P.ts(y_[:ns, :], R(py, py.h[:ns].rearrange("p n f -> p (n f)")), l_[:ns, ti, 1:2], None, ALU.mult)
                a1, a2, a3 = self.MOE.h[:, :], i_.h[:ns, ti:ti + 1], y_.h[:ns, :]
                P.op("pool", (lambda en, a1=a1, a2=a2, a3=a3: en.indirect_dma_start(
                    out=a1, out_offset=bass.IndirectOffsetOnAxis(ap=a2, axis=0), in_=a3, in_offset=None, compute_op=ALU.add)),
                    reads=[y_[:], i_[:], self.MOE[:]], writes=[self.MOE[:]], dma_key="fs")
        P.pop()
        P.push()
        md = self.load_mod([5])
        xt = [P.tile([128, 1024], F32, "xt") for _ in range(2)]
        mo = [P.tile([128, 1024], F32, "mo") for _ in range(2)]
        for i in range(NT):
            which = 0 if i < nctx else 1
            if last and which == 0:
                continue
            x_ = xt[i % 2]
            m_ = mo[i % 2]
            P.dma(x_[:], self.X[i * 128:(i + 1) * 128, :], key="cx%d" % (i % 2))
            P.dma(m_[:], self.MOE[i * 128:(i + 1) * 128, :], key="cm%d" % (i % 2), q="pool")
            P.tt(m_[:], m_[:], md[(which, 5)][:], ALU.mult)
            P.tt(x_[:], x_[:], m_[:], ALU.add, eng="pool")
            if last:
                j = i - nctx
                P.dma(self.out[j * 128:(j + 1) * 128, :], x_[:], key="co%d" % (i % 2))
            else:
                P.dma(self.X[i * 128:(i + 1) * 128, :], x_[:], key="co%d" % (i % 2))
        P.pop()


    def mla_layer(self, l, j):
        import os
        P, C = self.P, self.C
        T, NT = C["T"], self.NT
        P.push()
        wdn_d = P.din("mla_wdown%d" % j, [1024, 416])
        wuq_d = P.din("mla_wuq%d" % j, [256, 1536])
        wukv_d = P.din("mla_wukv%d" % j, [128, 2048])
        qag_d = P.din("mla_qag%d" % j, [256])
        kvag_d = P.din("mla_kvag%d" % j, [128])
        qg_d = P.din("mla_qg%d" % j, [96])
        kg_d = P.din("mla_kg%d" % j, [96])
        cosd = P.din("cosM", [T, 16])
        sind = P.din("sinM", [T, 16])
        stg = [P.tile([128, 2048], F32, "stg") for _ in range(2)]
        cnt = [0]
        wdn = P.tile([128, 8, 416], BF16, "wdn")
        self.load_w(wdn, wdn_d.h, 8, 416, stg, cnt)
        wuq = P.tile([128, 2, 1536], BF16, "wuq")
        self.load_w(wuq, wuq_d.h, 2, 1536, stg, cnt)
        wukv = P.tile([128, 1, 2048], BF16, "wukv")
        self.load_w(wukv, wukv_d.h, 1, 2048, stg, cnt)
        md = self.load_mod([0, 1])
        qag = P.tile([128, 256], F32, "qag")
        P.dma(qag[:], R(qag_d, qag_d.h[:].partition_broadcast(128)), key="g0", q="pool")
        kvag = P.tile([128, 128], F32, "kvag")
        P.dma(kvag[:], R(kvag_d, kvag_d.h[:].partition_broadcast(128)), key="g0", q="pool")
        qkg = P.tile([128, 32, 96], F32, "qkg")
        P.dma(qkg[:, 0:16, :], R(qg_d, qg_d.h[:].partition_broadcast(128).unsqueeze(1).broadcast_to([128, 16, 96])), key="g0", q="pool")
        P.dma(qkg[:, 16:32, :], R(kg_d, kg_d.h[:].partition_broadcast(128).unsqueeze(1).broadcast_to([128, 16, 96])), key="g0", q="pool")
        xt = [P.tile([128, 1024], F32, "xt") for _ in range(2)]
        cs = [P.tile([128, 2, 16], F32, "cs") for _ in range(2)]
        sq = P.tile([128, 32 * 96], F32, "sq")
        tmp = P.tile([128, 1024], F32, "tmp")
        hb = P.tile([128, 1024], BF16, "hb")
        hT = [P.tile([128, 8, 128], BF16, "hT") for _ in range(2)]
        small = [(P.tile([128, 1], F32, "ss"), P.tile([128, 1], F32, "rs")) for _ in range(2)]
        sa = [P.tile([128, 1], F32, "sa") for _ in range(4)]
        cqn = P.tile([128, 384], BF16, "cqn")
        cT_ = P.tile([128, 3, 128], BF16, "cT_")
        krs = P.tile([128, 32], F32, "krs")
        qk = P.tile([128, 32, 96], F32, "qk")
        ss32 = P.tile([128, 32], F32, "ss32")
        rs32 = P.tile([128, 32], F32, "rs32")
        t1 = P.tile([128, 32, 16], F32, "t1")
        t2 = P.tile([128, 32, 16], F32, "t2")
        t3 = P.tile([128, 32, 16], F32, "t3")
        t4 = P.tile([128, 32, 16], F32, "t4")
        qb = P.tile([128, 32, 96], BF16, "qb")
        qkT = [P.tile([96, 32, 128], BF16, "qkT") for _ in range(2)]
        va = [P.tile([128, 16, 65], BF16, "va") for _ in range(2)]
        for v_ in va:
            P.memset(v_[:], 1.0)
        pT = P.psum([128, 8, 128], BF16, "pT")
        plat = P.psum([128, 416], F32, "plat")
        pq = P.psum([128, 3, 512], F32, "pq")
        pkv = P.psum([128, 2, 512], F32, "pkv")
        X = self.xsrc(l)
        for i in range(NT):
            which = 0 if i < self.nctx else 1
            x_ = xt[i % 2]
            P.dma(x_[:], X[i * 128:(i + 1) * 128, :], key="x%d" % (i % 2))
            c_ = cs[i % 2]
            P.dma(c_[:, 0, :], cosd[i * 128:(i + 1) * 128, :], key="cs%d" % (i % 2), q="pool")
            P.dma(c_[:, 1, :], sind[i * 128:(i + 1) * 128, :], key="cs%d" % (i % 2), q="pool")
            self.front(x_, md[(which, 0)][:], md[(which, 1)][:], sq[:, :1024], small[i % 2], tmp, hb)
            h_ = hT[i % 2]
            self.transposeT(h_, hb, 8, pT)
            for k in range(8):
                P.mm(plat[:], h_[:, k, :], wdn[:, k, :], start=(k == 0), stop=(k == 7))
            P.act(sq[:, 0:256], plat[:, 0:256], AF.Square, accum_out=sa[0][:])
            self.rstd(sa[1][:], sa[0][:], 256)
            P.stt(cqn[:, 0:256], plat[:, 0:256], sa[1][:], qag[:], ALU.mult, ALU.mult)
            P.act(sq[:, 256:384], plat[:, 256:384], AF.Square, accum_out=sa[2][:])
            self.rstd(sa[3][:], sa[2][:], 128)
            P.stt(cqn[:, 256:384], plat[:, 256:384], sa[3][:], kvag[:], ALU.mult, ALU.mult)
            P.copy(krs[:], plat[:, 384:416], eng="act")
            STOP = float(os.environ.get("MK_STOP", "9"))
            if STOP <= 1:
                continue
            self.transposeT(cT_, cqn, 3, pT)
            if STOP <= 1.2:
                continue
            for n in range(3):
                for k in range(2):
                    P.mm(pq[:, n, :], cT_[:, k, :], wuq[:, k, n * 512:(n + 1) * 512], start=(k == 0), stop=(k == 1))
            qkflat = qk.h[:, 0:16, :].rearrange("p h d -> p (h d)")
            for n in range(3):
                P.copy(R(qk, qkflat[:, n * 512:(n + 1) * 512]), pq[:, n, :], eng=("act" if n % 2 == 0 else "dve"))
            if STOP <= 1.4:
                continue
            v_ = va[i % 2]
            for hh in range(2):
                for n in range(2):
                    P.mm(pkv[:, n, :], cT_[:, 2, :], wukv[:, 0, hh * 1024 + n * 512:hh * 1024 + (n + 1) * 512], start=True, stop=True)
                kv3 = pkv.h[:].rearrange("p n (h d) -> p (n h) d", d=128)
                P.copy(qk[:, 16 + hh * 8:16 + (hh + 1) * 8, 0:64], R(pkv, kv3[:, :, 0:64]))
                P.copy(v_[:, hh * 8:(hh + 1) * 8, 0:64], R(pkv, kv3[:, :, 64:128]), eng="act")
            if STOP <= 1.6:
                continue
            P.copy(qk[:, 16:32, 64:96], R(krs, krs.h[:].unsqueeze(1).broadcast_to([128, 16, 32])), eng="pool")
            if STOP <= 1.8:
                continue
            P.dma(self.V[i * 128:(i + 1) * 128, :, :], v_[:], key="vo%d" % (i % 2), q="pool")
            if STOP <= 2:
                continue
            P.act(R(sq, sq.h[:].rearrange("p (h d) -> p h d", d=96)), qk[:], AF.Square)
            P.reduce(ss32[:], R(sq, sq.h[:].rearrange("p (h d) -> p h d", d=96)))
            self.rstd(rs32[:], ss32[:], 96)
            P.tt(qk[:], qk[:], R(rs32, rs32.h[:].unsqueeze(2).broadcast_to([128, 32, 96])), ALU.mult)
            P.tt(qk[:], qk[:], qkg[:], ALU.mult, eng="pool")
            P.copy(qb[:, :, 0:64], qk[:, :, 0:64], eng="act")
            cosb = R(c_, c_.h[:, 0, :].unsqueeze(1).broadcast_to([128, 32, 16]))
            sinb = R(c_, c_.h[:, 1, :].unsqueeze(1).broadcast_to([128, 32, 16]))
            x1 = qk[:, :, 64:80]
            x2 = qk[:, :, 80:96]
            P.tt(t1[:], x1, cosb, ALU.mult)
            P.tt(t2[:], x2, sinb, ALU.mult, eng="pool")
            P.tt(qb[:, :, 64:80], t1[:], t2[:], ALU.subtract)
            P.tt(t3[:], x2, cosb, ALU.mult, eng="pool")
            P.tt(t4[:], x1, sinb, ALU.mult)
            P.tt(qb[:, :, 80:96], t3[:], t4[:], ALU.add, eng="pool")
            if STOP <= 3:
                continue
            q_ = qkT[i % 2]
            for b0 in range(0, 32, 8):
                for jj in range(8):
                    P.transpose(pT[:96, jj, :], qb[:, b0 + jj, :], self.identb[:])
                P.copy(q_[:, b0:b0 + 8, :], pT[:96, :, :], eng=("dve" if (b0 // 8) % 2 == 0 else "act"))
            P.dma(R(self.QT, self.QT.h[:, :, i * 128:(i + 1) * 128].rearrange("m d t -> d m t")), q_[:, 0:16, :], key="qo%d" % (i % 2))
            P.dma(R(self.KT, self.KT.h[:, :, i * 128:(i + 1) * 128].rearrange("m d t -> d m t")), q_[:, 16:32, :], key="qo%d" % (i % 2))
        P.pop()
        import os
        if os.environ.get("MK_DUMP"):
            for nm, src in (("dQT", self.QT), ("dKT", self.KT)):
                dd = P.dout(nm, [16, 96, T], BF16)
                for m in range(16):
                    P.dma(dd[m], src[m], key="dump")
            dd = P.dout("dV", [T, 16, 65], BF16)
            for i in range(NT):
                P.dma(dd[i * 128:(i + 1) * 128], self.V[i * 128:(i + 1) * 128], key="dump")
            self.P.barrier()
            return
        if "attn" not in os.environ.get("MK_SKIP", ""):
            attn_core(P, C, self.QT, self.KT, self.V, self.O, 16, 96, lambda m: [m], lambda m: [m], 96 ** -0.5, self.identf)
        P.push()
        S = self.post_alloc(l, "mla_wout%d" % j)
        ot = [P.tile([128, 16, 65], F32, "ot") for _ in range(2)]
        rd = P.tile([128, 16], F32, "rd")
        ob = [P.tile([128, 1024], BF16, "ob") for _ in range(2)]
        for i in range(NT):
            o_ = ot[i % 2]
            P.dma(o_[:], self.O[i * 128:(i + 1) * 128, 0:16, :], key="fo%d" % (i % 2))
            P.recip(rd[:], o_[:, :, 64])
            o2 = ob[i % 2]
            P.tt(R(o2, o2.h[:].rearrange("p (h d) -> p h d", d=64)), o_[:, :, 0:64], R(rd, rd.h[:].unsqueeze(2).broadcast_to([128, 16, 64])), ALU.mult)
            self.post(l, S, i, o2)
        P.pop()


    def gdn_layer(self, l, j):
        import os
        P, C = self.P, self.C
        T, NT, CTX = C["T"], self.NT, C["CTX"]
        NC = T // 64
        ncc = CTX // 64
        if "PRE" not in self.__dict__:
            self.PRE = P.dscratch("PRE", [24, 128, T], F32)
            self.QKVT = P.dscratch("QKVT", [24, 128, T], BF16)
            self.Zd = P.dscratch("Zd", [T, 1024], F32)
            self.GB = P.dscratch("GB", [T, 32], F32)
            self.OG = P.dscratch("OG", [2, T, 1024], F32)
        PRE, QKVT, Zd, GB, OG = self.PRE, self.QKVT, self.Zd, self.GB, self.OG
        P.push()
        win_d = P.din("gdn_win%d" % j, [1024, 4128])
        alog_d = P.din("gdn_alog%d" % j, [16])
        dtb_d = P.din("gdn_dtb%d" % j, [16])
        w = P.tile([128, 8, 4128], BF16, "gwin")
        stg = [P.tile([128, 2048], F32, "stg") for _ in range(2)]
        self.load_w(w, win_d.h, 8, 4128, stg, [0])
        md = self.load_mod([0, 1])
        nega = P.tile([128, 16], F32, "nega")
        P.dma(nega[:], R(alog_d, alog_d.h[:].partition_broadcast(128)), key="g0", q="pool")
        P.act(nega[:], nega[:], AF.Exp)
        P.ts(nega[:], nega[:], -1.0, None, ALU.mult)
        dtb = P.tile([128, 16], F32, "dtb")
        P.dma(dtb[:], R(dtb_d, dtb_d.h[:].partition_broadcast(128)), key="g0", q="pool")
        xt = [P.tile([128, 1024], F32, "xt") for _ in range(2)]
        sq = P.tile([128, 1024], F32, "sq")
        tmp = P.tile([128, 1024], F32, "tmp")
        hb = P.tile([128, 1024], BF16, "hb")
        hT = [P.tile([128, 8, 128], BF16, "hT") for _ in range(2)]
        small = [(P.tile([128, 1], F32, "ss"), P.tile([128, 1], F32, "rs")) for _ in range(2)]
        pre = [P.tile([128, 24, 128], F32, "pre") for _ in range(1)]
        zt = [P.tile([128, 1024], F32, "zt") for _ in range(1)]
        gb = [P.tile([128, 32], F32, "gb") for _ in range(2)]
        t16 = P.tile([128, 16], F32, "t16")
        pT = P.psum([128, 8, 128], BF16, "pT")
        pf = [P.psum([128, 4, 128], F32, "pf") for _ in range(2)]
        pz = P.psum([128, 3, 512], F32, "pz")
        X = self.xsrc(l)
        for i in range(NT):
            which = 0 if i < self.nctx else 1
            x_ = xt[i % 2]
            P.dma(x_[:], X[i * 128:(i + 1) * 128, :], key="x%d" % (i % 2))
            self.front(x_, md[(which, 0)][:], md[(which, 1)][:], sq, small[i % 2], tmp, hb)
            h_ = hT[i % 2]
            self.transposeT(h_, hb, 8, pT)
            pr_ = pre[0]
            for g4 in range(6):
                pf_ = pf[g4 % 2]
                for c4 in range(4):
                    cc = g4 * 4 + c4
                    for k in range(8):
                        P.mm(pf_[:, c4, :], w[:, k, cc * 128:(cc + 1) * 128], h_[:, k, :], start=(k == 0), stop=(k == 7))
                P.copy(pr_[:, g4 * 4:(g4 + 1) * 4, :], pf_[:], eng=("act" if g4 % 2 == 0 else "dve"))
            P.dma(R(PRE, PRE.h[:, :, i * 128:(i + 1) * 128].rearrange("c p t -> p c t")), pr_[:], key="pre%d" % (i % 2))
            for n in range(2):
                for k in range(8):
                    P.mm(pz[:, n, :], h_[:, k, :], w[:, k, 3072 + n * 512:3072 + (n + 1) * 512], start=(k == 0), stop=(k == 7))
            for k in range(8):
                P.mm(pz[:, 2, 0:32], h_[:, k, :], w[:, k, 4096:4128], start=(k == 0), stop=(k == 7))
            z_ = zt[0]
            P.copy(R(z_, z_.h[:].rearrange("p (n f) -> p n f", n=2)), pz[:, 0:2, :], eng="act")
            P.dma(Zd[i * 128:(i + 1) * 128, :], z_[:], key="zo%d" % (i % 2), q="pool")
            g_ = gb[i % 2]
            P.tt(t16[:], pz[:, 2, 0:16], dtb[:], ALU.add)
            P.act(t16[:], t16[:], AF.Exp)
            P.ts(t16[:], t16[:], 1.0, None, ALU.add)
            P.act(t16[:], t16[:], AF.Ln)
            P.tt(g_[:, 0:16], t16[:], nega[:], ALU.mult)
            P.act(g_[:, 16:32], pz[:, 2, 16:32], AF.Exp, scale=-1.0)
            P.ts(g_[:, 16:32], g_[:, 16:32], 1.0, None, ALU.add)
            P.act(g_[:, 16:32], g_[:, 16:32], AF.Ln)
            P.ts(g_[:, 16:32], g_[:, 16:32], -1.0, None, ALU.mult)
            P.dma(GB[i * 128:(i + 1) * 128, :], g_[:], key="go%d" % (i % 2), q="pool")
        P.pop()
        P.push()
        cw_d = P.din("gdn_cw%d" % j, [128, 24, 5])
        cw = P.tile([128, 24, 5], F32, "cw")
        P.dma(cw[:], cw_d[:], key="g0")
        onesb = P.tile([128, 128], BF16, "onesb")
        P.memset(onesb[:], 1.0)
        xt = P.tile([128, T], F32, "cx")
        acc = P.tile([128, T], F32, "cacc")
        sqb = P.tile([128, T], BF16, "csq")
        ob = [P.tile([128, T], BF16, "cob") for _ in range(2)]
        rsb = [P.tile([128, 512], F32, "crs") for _ in range(2)]
        pss = [P.psum([128, 512], F32, "pss") for _ in range(2)]
        segs = [(0, CTX), (CTX, T)]
        for cc in range(24):
            P.dma(xt[:], PRE[cc], key="cx")
            P.ts(acc[:], xt[:], cw[:, cc, 2:3], None, ALU.mult)
            for off in (-2, -1, 1, 2):
                for (s0, s1) in segs:
                    a = max(s0, s0 - off)
                    b = min(s1, s1 - off)
                    P.stt(acc[:, a:b], xt[:, a + off:b + off], cw[:, cc, off + 2:off + 3], acc[:, a:b], ALU.mult, ALU.add)
            P.act(acc[:], acc[:], AF.Silu)
            o_ = ob[cc % 2]
            if cc < 16:
                P.act(sqb[:], acc[:], AF.Square)
                nb = 0
                for b0 in range(0, T, 512):
                    bw = min(512, T - b0)
                    ps_ = pss[nb % 2]
                    r_ = rsb[nb % 2]
                    nb += 1
                    P.mm(ps_[:, :bw], onesb[:], sqb[:, b0:b0 + bw])
                    P.ts(r_[:, :bw], ps_[:, :bw], EPS, None, ALU.add)
                    P.act(r_[:, :bw], r_[:, :bw], AF.Sqrt)
                    P.recip(r_[:, :bw], r_[:, :bw])
                    if cc < 8:
                        P.stt(o_[:, b0:b0 + bw], acc[:, b0:b0 + bw], 128 ** -0.5, r_[:, :bw], ALU.mult, ALU.mult)
                    else:
                        P.tt(o_[:, b0:b0 + bw], acc[:, b0:b0 + bw], r_[:, :bw], ALU.mult, eng="pool")
            else:
                P.copy(o_[:], acc[:], eng="pool")
            P.dma(QKVT[cc], o_[:], key="co%d" % (cc % 2), q="pool")
        P.pop()
        P.push()
        gm_d = P.din("gmask", [2, 4, 64, 64])
        idr_d = P.din("identrep", [64, 8, 64])
        identrep = P.tile([64, 8, 64], F32, "identrep")
        P.dma(identrep[:], idr_d[:], key="g0")
        ones64 = P.tile([64, 128], F32, "ones64")
        P.memset(ones64[:], 1.0)
        banks = [P.psum([128, 512], F32, "bank") for _ in range(7)]
        bankb = P.psum([128, 1024], BF16, "bankb")
        bi = [0]

        def nbank():
            b = banks[bi[0] % 7]
            bi[0] += 1
            return b
        gtok = P.tile([64, NC, 32], F32, "gtok")
        P.dma(gtok[:], R(GB, GB.h.rearrange("(c p) f -> p c f", p=64)), key="g1")
        S = P.tile([128, 8, 128], F32, "S")
        Sb = P.tile([128, 8, 128], BF16, "Sb")
        kq = [P.tile([128, 8, 2, 64], BF16, "kq") for _ in range(2)]
        vT = [P.tile([128, 8, 64], BF16, "vT") for _ in range(2)]
        dcol = P.tile([64, NC, 8], F32, "dcol")
        dbcol = P.tile([64, NC, 8], F32, "dbcol")
        ed = P.tile([64, NC, 8], F32, "ed")
        be = P.tile([64, NC, 8], F32, "be")
        ed2 = P.tile([64, NC, 8], F32, "ed2")
        bet = P.tile([64, NC, 8], F32, "bet")
        lastbc = P.tile([128, NC, 8], F32, "lastbc")
        Lm = P.tile([64, 64], F32, "Lm")
        negi = P.tile([64, 64], F32, "negi")
        negs = P.tile([64, 64], F32, "negs")
        poss = P.tile([64, 64], F32, "poss")
        Dg = P.tile([64, 2, 8, 64], F32, "Dg")
        dfa = P.tile([64, 8, 64], F32, "dfa")
        dfb = P.tile([64, 8, 64], F32, "dfb")
        dfc = P.tile([64, 8, 64], F32, "dfc")
        kkq = P.tile([64, 8, 128], F32, "kkq")
        X_ = [P.tile([64, 8, 64], F32, "X") for _ in range(2)]
        Y_ = [P.tile([64, 8, 64], F32, "Y") for _ in range(2)]
        Z_ = [P.tile([64, 8, 64], F32, "Z") for _ in range(2)]
        qkm = P.tile([64, 8, 64], BF16, "qkm")
        ktok = P.tile([64, 8, 128], BF16, "ktok")
        kdec = P.tile([64, 8, 128], BF16, "kdec")
        bv = P.tile([64, 8, 128], F32, "bv")
        rr = P.tile([64, 4, 128], F32, "rr")
        vn = P.tile([64, 4, 128], BF16, "vn")
        ot = [P.tile([64, 8, 128], F32, "ot") for _ in range(2)]
        o1s = P.tile([64, 4, 128], F32, "o1s")

        def b3(t, n):
            return R(t[0], t[0].h[:, t[1], :].unsqueeze(2).broadcast_to([64, 8, n]))

        def v3(bank, p, a, b):
            return R(bank, bank.h[:p, :a * b].rearrange("p (a b) -> p a b", a=a))
        for d in range(2):
            P.dma(Lm[:], gm_d[d, 0], key="g2")
            P.dma(negi[:], gm_d[d, 1], key="g2")
            P.dma(negs[:], gm_d[d, 2], key="g2")
            P.dma(poss[:], gm_d[d, 3], key="g2")
            gsl = R(gtok, gtok.h[:, :, d * 8:(d + 1) * 8])
            lbs = R(gtok, gtok.h[:, :, 16 + d * 8:16 + (d + 1) * 8])
            gflat = P.tile([64, NC, 8], F32, "gflat") if d == 0 else gflat
            P.copy(gflat[:], gsl)
            gf2 = gflat.h[:].rearrange("p c h -> p (c h)")
            for n0 in range(0, NC * 8, 512):
                nw = min(512, NC * 8 - n0)
                bk = nbank()
                P.mm(bk[:64, :nw], Lm[:], R(gflat, gf2[:, n0:n0 + nw]))
                P.copy(R(dcol, dcol.h[:].rearrange("p c h -> p (c h)")[:, n0:n0 + nw]), bk[:64, :nw])
                bk = nbank()
                P.mm(bk[:, :nw], ones64[:], R(gflat, gf2[:, n0:n0 + nw]))
                P.copy(R(lastbc, lastbc.h[:].rearrange("p c h -> p (c h)")[:, n0:n0 + nw]), bk[:, :nw], eng="act")
            P.tt(ed2[:], lastbc[:64], dcol[:], ALU.subtract)
            P.act(ed2[:], ed2[:], AF.Exp)
            P.act(lastbc[:], lastbc[:], AF.Exp)
            P.act(ed[:], dcol[:], AF.Exp)
            P.tt(dbcol[:], dcol[:], lbs, ALU.add)
            P.act(be[:], dbcol[:], AF.Exp)
            P.act(bet[:], lbs, AF.Exp)
            P.memset(S[:], 0.0)
            P.memset(Sb[:], 0.0)
            if d == 0:
                order = list(range(NC))
            else:
                order = list(range(ncc - 1, -1, -1)) + list(range(NC - 1, ncc - 1, -1))
            for ci, c in enumerate(order):
                t0 = c * 64
                kq_ = kq[ci % 2]
                v_ = vT[ci % 2]
                P.dma(kq_[:, :, 0, :], R(QKVT, QKVT.h[8:16, :, t0:t0 + 64].rearrange("h p t -> p h t")), key="lk%d" % (ci % 2))
                P.dma(kq_[:, :, 1, :], R(QKVT, QKVT.h[0:8, :, t0:t0 + 64].rearrange("h p t -> p h t")), key="lk%d" % (ci % 2))
                P.dma(v_[:], R(QKVT, QKVT.h[16:24, :, t0:t0 + 64].rearrange("h p t -> p h t")), key="lv%d" % (ci % 2), q="pool")
                for hg in range(2):
                    bk = nbank()
                    for h4 in range(4):
                        h = hg * 4 + h4
                        P.mm(bk[:64, h4 * 128:(h4 + 1) * 128], kq_[:, h, 0, :], R(kq_, kq_.h[:, h, :, :].rearrange("p a t -> p (a t)")))
                    P.copy(kkq[:, hg * 4:(hg + 1) * 4, :], v3(bk, 64, 4, 128), eng="act")
                P.tt(Dg[:, 0], identrep[:], b3((dcol, c), 64), ALU.mult)
                P.tt(Dg[:, 1], identrep[:], b3((dbcol, c), 64), ALU.mult, eng="pool")
                bk0 = nbank()
                P.mm(bk0[:64, :], ones64[:, :64], R(Dg, Dg.h[:, 0].rearrange("p h i -> p (h i)")))
                bk1 = nbank()
                P.mm(bk1[:64, :], ones64[:, :64], R(Dg, Dg.h[:, 1].rearrange("p h i -> p (h i)")))
                P.tt(dfa[:], v3(bk0, 64, 8, 64), b3((dcol, c), 64), ALU.subtract)
                P.tt(dfa[:], dfa[:], R(negi, negi.h[:].unsqueeze(1).broadcast_to([64, 8, 64])), ALU.add, eng="pool")
                P.act(dfa[:], dfa[:], AF.Exp)
                P.tt(qkm[:], kkq[:, :, 64:128], dfa[:], ALU.mult)
                P.tt(dfb[:], v3(bk1, 64, 8, 64), b3((dcol, c), 64), ALU.subtract)
                P.tt(dfb[:], dfb[:], R(negs, negs.h[:].unsqueeze(1).broadcast_to([64, 8, 64])), ALU.add, eng="pool")
                P.act(dfb[:], dfb[:], AF.Exp)
                X0 = X_[0]
                P.stt(X0[:], kkq[:, :, 0:64], -1.0, dfb[:], ALU.mult, ALU.mult)
                P.tt(dfc[:], v3(bk0, 64, 8, 64), b3((dbcol, c), 64), ALU.subtract)
                P.tt(dfc[:], dfc[:], R(poss, poss.h[:].unsqueeze(1).broadcast_to([64, 8, 64])), ALU.add, eng="pool")
                P.act(dfc[:], dfc[:], AF.Exp, scale=-1.0)
                Y0 = Y_[0]
                P.stt(Y0[:], kkq[:, :, 0:64], -1.0, dfc[:], ALU.mult, ALU.mult)
                Zc = Z_[0]
                P.tt(Zc[:], X0[:], identrep[:], ALU.add, eng="pool")
                Xc, Yc = X0, Y0
                for lev in range(1, 6):
                    Yn = Y_[lev % 2]
                    bk = nbank()
                    for h in range(8):
                        P.mm(bk[:64, h * 64:(h + 1) * 64], Xc[:, h, :], Yc[:, h, :])
                    if lev < 5:
                        Xn = X_[lev % 2]
                        bk2 = nbank()
                        for h in range(8):
                            P.mm(bk2[:64, h * 64:(h + 1) * 64], Yc[:, h, :], Xc[:, h, :])
                        P.copy(Xn[:], v3(bk2, 64, 8, 64), eng="act")
                    P.copy(Yn[:], v3(bk, 64, 8, 64))
                    Zn = Z_[lev % 2]
                    bk3 = nbank()
                    for h in range(8):
                        P.mm(bk3[:64, h * 64:(h + 1) * 64], Yn[:, h, :], Zc[:, h, :])
                    P.tt(Zn[:], Zc[:], v3(bk3, 64, 8, 64), ALU.add)
                    Zc = Zn
                    Yc = Yn
                    if lev < 5:
                        Xc = Xn
                for h in range(8):
                    P.transpose(bankb[:64, h * 128:(h + 1) * 128], kq_[:, h, 0, :], self.identb[:])
                kt3 = R(bankb, bankb.h[:64, :].rearrange("p (h d) -> p h d", h=8))
                P.tt(kdec[:], kt3, b3((ed2, c), 128), ALU.mult)
                for h in range(8):
                    P.transpose(bankb[:64, h * 128:(h + 1) * 128], v_[:, h, :], self.identb[:])
                P.tt(bv[:], kt3, b3((bet, c), 128), ALU.mult)
                o_ = ot[ci % 2]
                for hg in range(2):
                    hs = slice(hg * 4, (hg + 1) * 4)
                    bk = nbank()
                    for h4 in range(4):
                        h = hg * 4 + h4
                        P.mm(bk[:64, h4 * 128:(h4 + 1) * 128], kq_[:, h, 0, :], Sb[:, h, :])
                    be4 = R(be, be.h[:, c, hs].unsqueeze(2).broadcast_to([64, 4, 128]))
                    P.tt(rr[:], v3(bk, 64, 4, 128), be4, ALU.mult)
                    P.tt(rr[:], bv[:, hs, :], rr[:], ALU.subtract, eng="pool")
                    bk = nbank()
                    for h4 in range(4):
                        h = hg * 4 + h4
                        P.mm(bk[:64, h4 * 128:(h4 + 1) * 128], Zc[:, h, :], rr[:, h4, :])
                    P.copy(vn[:], v3(bk, 64, 4, 128), eng="act")
                    bk = nbank()
                    for h4 in range(4):
                        h = hg * 4 + h4
                        P.mm(bk[:64, h4 * 128:(h4 + 1) * 128], kq_[:, h, 1, :], Sb[:, h, :])
                    ed4 = R(ed, ed.h[:, c, hs].unsqueeze(2).broadcast_to([64, 4, 128]))
                    P.tt(o1s[:], v3(bk, 64, 4, 128), ed4, ALU.mult)
                    bk = nbank()
                    for h4 in range(4):
                        h = hg * 4 + h4
                        P.mm(bk[:64, h4 * 128:(h4 + 1) * 128], qkm[:, h, :], vn[:, h4, :])
                    P.tt(o_[:, hs, :], o1s[:], v3(bk, 64, 4, 128), ALU.add)
                    bk = nbank()
                    for h4 in range(4):
                        h = hg * 4 + h4
                        P.mm(bk[:, h4 * 128:(h4 + 1) * 128], kdec[:, h, :], vn[:, h4, :])
                    l4 = R(lastbc, lastbc.h[:, c, hs].unsqueeze(2).broadcast_to([128, 4, 128]))
                    P.tt(S[:, hs, :], S[:, hs, :], l4, ALU.mult, eng="pool")
                    P.tt(S[:, hs, :], S[:, hs, :], v3(bk, 128, 4, 128), ALU.add)
                    P.copy(Sb[:, hs, :], S[:, hs, :], eng="act")
                P.dma(OG[d, t0:t0 + 64, :], R(o_, o_.h[:].rearrange("p h d -> p (h d)")), key="og%d" % (ci % 2))
        P.pop()
        P.push()
        S2 = self.post_alloc(l, "gdn_wout%d" % j)
        og_d = P.din("gdn_og%d" % j, [128])
        ogn = P.tile([128, 8, 128], F32, "ogn")
        P.dma(ogn[:], R(og_d, og_d.h[:].partition_broadcast(128).unsqueeze(1).broadcast_to([128, 8, 128])), key="fl")
        ot2 = [P.tile([128, 8, 128], F32, "ot2") for _ in range(2)]
        zt2 = [P.tile([128, 1024], F32, "zt2") for _ in range(2)]
        sqd = P.tile([128, 8, 128], F32, "sqd")
        ss8 = P.tile([128, 8], F32, "ss8")
        rs8 = P.tile([128, 8], F32, "rs8")
        ob2 = [P.tile([128, 1024], BF16, "ob2") for _ in range(2)]
        for i in range(NT):
            o_ = ot2[i % 2]
            z_ = zt2[i % 2]
            P.dma(R(o_, o_.h[:].rearrange("p h d -> p (h d)")), OG[0, i * 128:(i + 1) * 128, :], key="fo%d" % (i % 2))
            P.dma(R(sqd, sqd.h[:].rearrange("p h d -> p (h d)")), OG[1, i * 128:(i + 1) * 128, :], key="fo%d" % (i % 2))
            P.dma(z_[:], Zd[i * 128:(i + 1) * 128, :], key="fz%d" % (i % 2), q="pool")
            P.tt(o_[:], o_[:], sqd[:], ALU.add, eng="pool")
            P.act(sqd[:], o_[:], AF.Square)
            P.reduce(ss8[:], sqd[:])
            self.rstd(rs8[:], ss8[:], 128)
            P.tt(o_[:], o_[:], R(rs8, rs8.h[:].unsqueeze(2).broadcast_to([128, 8, 128])), ALU.mult)
            P.tt(o_[:], o_[:], ogn[:], ALU.mult, eng="pool")
            P.act(z_[:], z_[:], AF.Silu)
            o2 = ob2[i % 2]
            P.tt(o2[:], R(o_, o_.h[:].rearrange("p h d -> p (h d)")), z_[:], ALU.mult)
            self.post(l, S2, i, o2)
        P.pop()


def rope_tables(T, CTX, GRID_W, dim):
    nf = dim // 4
    inv = 10000.0 ** (-np.arange(nf, dtype=np.float32) / nf)
    n = T - CTX
    rows = n // GRID_W
    r = np.repeat(np.arange(rows, dtype=np.float32), GRID_W)
    c = np.tile(np.arange(GRID_W, dtype=np.float32), rows)
    ang = np.concatenate([r[:, None] * inv, c[:, None] * inv], axis=-1).astype(np.float32)
    cos = np.ones((T, dim // 2), np.float32)
    sin = np.zeros((T, dim // 2), np.float32)
    cos[CTX:] = np.cos(ang)
    sin[CTX:] = np.sin(ang)
    return cos, sin


def build_program(C):
    P = Prog()
    net = Net(P, C)
    import os
    for l in range(int(os.environ.get("MK_L0", "0")), C["DEPTH"]):
        last = l == C["DEPTH"] - 1
        j = l // 3
        kind = l % 3
        import os
        if os.environ.get("MK_ALLDA"):
            kind, j = 0, 0
        net.mod_phase(l)
        if kind == 0:
            net.da_proj(l, j)
            attn_core_da(P, C, net.QT, net.KT, net.V2, net.O2, 0.125)
            net.da_finish(l, j)
        elif kind == 1:
            net.mla_layer(l, j)
        else:
            net.gdn_layer(l, j)
        if os.environ.get("MK_DUMP") and l == 1:
            break
        net.moe_phase(l, last)
    return P, net


def host_inputs(C, inp, b):
    T, CTX, SEQ = C["T"], C["CTX"], C["SEQ"]
    NT = T // 128
    f32 = np.float32
    d = {}
    d["ident"] = np.eye(128, dtype=f32)
    d["xall"] = np.ascontiguousarray(np.concatenate([inp["ctx"][b], inp["x"][b]], axis=0))
    cc = np.stack([inp["c"][b], inp["c_ctx"]], axis=-1)
    d["cT"] = np.ascontiguousarray(cc.reshape(8, 128, 2).transpose(1, 0, 2))
    cosD, sinD = rope_tables(T, CTX, C["GRID_W"], 64)
    d["cosD"], d["sinD"] = cosD, sinD
    cosM, sinM = rope_tables(T, CTX, C["GRID_W"], 32)
    d["cosM"], d["sinM"] = cosM, sinM
    U = np.triu(np.ones((128, 128), f32), 1)
    d["U"] = U
    d["tokidx"] = (np.arange(NT)[None, :] * 128 + np.arange(128)[:, None]).astype(f32)
    caps = [2 * CTX // 16, 2 * SEQ // 16]
    ecap = np.zeros((128, 2, 16), f32)
    base = 0
    for si in range(2):
        ecap[:, si, :] = base + np.arange(16) * caps[si]
        base += 16 * caps[si]
    d["ecap"] = ecap
    ipc = np.zeros((128, NT, 2), f32)
    ipc[:, :, 0] = np.arange(NT)[None, :]
    ipc[:, :, 1] = np.arange(128)[:, None]
    d["ipc"] = ipc
    d["iota"] = np.ascontiguousarray(np.broadcast_to(np.arange(1024, dtype=f32)[None, :], (128, 1024)))
    ii = np.arange(64)
    gm = np.zeros((2, 4, 64, 64), f32)
    NEG = -1.0e9
    le = (ii[:, None] <= ii[None, :]); lt = (ii[:, None] < ii[None, :])
    ge = (ii[:, None] >= ii[None, :]); gt = (ii[:, None] > ii[None, :])
    gm[0, 0] = le; gm[0, 1] = np.where(le, 0, NEG); gm[0, 2] = np.where(lt, 0, NEG); gm[0, 3] = np.where(gt, 0, -NEG)
    gm[1, 0] = ge; gm[1, 1] = np.where(ge, 0, NEG); gm[1, 2] = np.where(gt, 0, NEG); gm[1, 3] = np.where(lt, 0, -NEG)
    d["gmask"] = gm
    d["identrep"] = np.ascontiguousarray(np.broadcast_to(np.eye(64, dtype=f32)[:, None, :], (64, 8, 64)))
    d["dump"] = (base + np.arange(128)).astype(f32).reshape(128, 1)
    for l in range(C["DEPTH"]):
        d["ada_w%d" % l] = inp["ada_w"][l]
        d["ada_b%d" % l] = inp["ada_b"][l]
        d["ng%d" % l] = inp["norm_g"][l]
        d["rt%d" % l] = inp["moe_router"][l]
        d["wg%d" % l] = inp["moe_w_gate"][l]
        d["wu%d" % l] = inp["moe_w_up"][l]
        d["wd%d" % l] = inp["moe_w_down"][l]
        j = l // 3
        kind = l % 3
        import os
        if os.environ.get("MK_ALLDA"):
            kind, j = 0, 0
        if kind == 0:
            d["da_win%d" % j] = inp["da_w_in"][j]
            d["da_qg%d" % j] = inp["da_q_gain"][j]
            d["da_kg%d" % j] = inp["da_k_gain"][j]
            d["da_lam%d" % j] = np.ascontiguousarray(inp["da_lambda"][j].reshape(256))
            d["da_sg%d" % j] = inp["da_sub_gain"][j]
            d["da_wout%d" % j] = inp["da_w_out"][j]
        elif kind == 2:
            d["gdn_win%d" % j] = inp["gdn_w_in"][j]
            d["gdn_alog%d" % j] = np.ascontiguousarray(inp["gdn_a_log"][j].reshape(16))
            d["gdn_dtb%d" % j] = np.ascontiguousarray(inp["gdn_dt_bias"][j].reshape(16))
            d["gdn_cw%d" % j] = np.ascontiguousarray(inp["gdn_conv_w"][j].reshape(5, 24, 128).transpose(2, 1, 0))
            d["gdn_og%d" % j] = inp["gdn_o_gain"][j]
            d["gdn_wout%d" % j] = inp["gdn_w_out"][j]
        if kind == 1:
            d["mla_wdown%d" % j] = inp["mla_w_down"][j]
            d["mla_wuq%d" % j] = inp["mla_w_uq"][j]
            d["mla_wukv%d" % j] = inp["mla_w_ukv"][j]
            d["mla_qag%d" % j] = inp["mla_q_a_gain"][j]
            d["mla_kvag%d" % j] = inp["mla_kv_a_gain"][j]
            d["mla_qg%d" % j] = inp["mla_q_gain"][j]
            d["mla_kg%d" % j] = inp["mla_k_gain"][j]
            d["mla_wout%d" % j] = inp["mla_w_out"][j]
    return d


def kernel(**inp):
    inp = {k: np.asarray(v) for k, v in inp.items()}
    B, SEQ, D = inp["x"].shape
    CTX = inp["ctx"].shape[1]
    C = dict(SEQ=SEQ, CTX=CTX, T=SEQ + CTX, GRID_W=64, DEPTH=inp["ada_w"].shape[0], FF=inp["moe_w_gate"].shape[-1])
    P, net = build_program(C)
    nc = P.build()
    names = set()
    import concourse.mybir as mb
    in_maps = []
    for b in range(B):
        d = host_inputs(C, inp, b)
        in_maps.append(d)
    used = [a.memorylocations[0].name for a in nc.allocations if isinstance(a, mb.MemoryLocationSet) and a.kind == "ExternalInput"]
    in_maps = [{k: np.ascontiguousarray(m[k]) for k in used if k in m} for m in in_maps]
    res = run_bass_kernel_spmd(nc, in_maps, core_ids=list(range(B)))
    import os
    if os.environ.get("MK_DUMP"):
        return res.results
    return np.stack([res.results[b]["out"] for b in range(B)], axis=0).astype(np.float32)
```

```python
import numpy as np
import concourse.bass as bass
import concourse.mybir as mybir
from concourse.bass_utils import run_bass_kernel_spmd

F32 = mybir.dt.float32
BF16 = mybir.dt.bfloat16
I32 = mybir.dt.int32
ALU = mybir.AluOpType
AF = mybir.ActivationFunctionType
AX = mybir.AxisListType

SAME_ENGINE_SYNC = True
SEM_EPOCH = 30000
DMA_EPOCH = 2000


class Ref:
    __slots__ = ("ap", "key")

    def __init__(self, ap, key):
        self.ap = ap
        self.key = key

    def __getitem__(self, idx):
        return Ref(self.ap[idx], self.key)


class TT:
    def __init__(self, handle, key, is_ap=False):
        self.h = handle
        self.key = key

    def __getitem__(self, idx):
        return Ref(self.h[idx], self.key)

    def ref(self, ap):
        return Ref(ap, self.key)


class Prog:
    ENG = ("pe", "act", "dve", "pool", "sp")

    def __init__(self):
        self.nc = bass.Bass("TRN2", target_bir_lowering=False)
        self.ops = []
        self.last_w = {}
        self.readers = {}
        self.ctxs = []
        self.n_t = 0
        self.outs = []
        self.scopes = []
        self.freed = []

    def dram(self, name, shape, dtype, kind):
        t = self.nc.dram_tensor(name, list(shape), dtype, kind=kind)
        return TT(t.ap(), "D:" + name)

    def din(self, name, shape, dtype=F32):
        if not hasattr(self, "_dins"):
            self._dins = {}
        if name not in self._dins:
            self._dins[name] = self.dram(name, shape, dtype, "ExternalInput")
        return self._dins[name]

    def dout(self, name, shape, dtype=F32):
        self.outs.append(name)
        return self.dram(name, shape, dtype, "ExternalOutput")

    def dscratch(self, name, shape, dtype=F32):
        return self.dram(name, shape, dtype, "Internal")

    def tile(self, shape, dtype=F32, name=None):
        self.n_t += 1
        name = (name or "t") + "_%d" % self.n_t
        g = self.nc.sbuf_tensor(name, list(shape), dtype)
        h = g.__enter__()
        (self.scopes[-1] if self.scopes else self.ctxs).append(g)
        return TT(h, "S:" + name)

    def psum(self, shape, dtype=F32, name=None):
        self.n_t += 1
        name = (name or "p") + "_%d" % self.n_t
        g = self.nc.psum_tensor(name, list(shape), dtype)
        h = g.__enter__()
        (self.scopes[-1] if self.scopes else self.ctxs).append(g)
        return TT(h, "P:" + name)

    def push(self):
        self.scopes.append([])

    def pop(self):
        self.barrier()
        for g in reversed(self.scopes.pop()):
            g.__exit__(None, None, None)

    def barrier(self):
        n = len(self.ops)
        if n == 0:
            return
        last = {}
        dl = {}
        for i, o in enumerate(self.ops):
            if o["dma"] is not None:
                dl[o["dma"]] = i
            else:
                last[o["eng"]] = i
        deps = set(last.values()) | set(dl.values())
        for e in self.ENG:
            self.ops.append(dict(eng=e, fn=(lambda en: en.nop()), deps=set(deps), dma=None))
        if hasattr(self, "_pkeys"):
            self._pkeys = {}

    def op(self, eng, fn, reads=(), writes=(), dma_key=None, extra_deps=()):
        i = len(self.ops)
        deps = set(extra_deps)
        rk = [r.key if isinstance(r, Ref) else r for r in reads]
        wk = [w.key if isinstance(w, Ref) else w for w in writes]
        for k in rk:
            if k in self.last_w:
                deps.add(self.last_w[k])
            if isinstance(k, str) and k.startswith("P:"):
                for r in self.readers.get(k, ()):
                    if self.ops[r]["eng"] != eng:
                        deps.add(r)
        for k in wk:
            if k in self.last_w:
                deps.add(self.last_w[k])
            for r in self.readers.get(k, ()):
                deps.add(r)
        deps.discard(i)
        for k in rk:
            self.readers.setdefault(k, []).append(i)
        for k in wk:
            self.last_w[k] = i
            self.readers[k] = []
        if dma_key is not None:
            if not hasattr(self, "_dcount"):
                self._dcount = {}
                self._pkeys = {}
            if dma_key not in self._pkeys:
                self._pkeys[dma_key] = "slot%d" % len(self._pkeys)
            dma_key = self._pkeys[dma_key]
            c = self._dcount.get(dma_key, 0)
            self._dcount[dma_key] = c + 1
            dma_key = (dma_key, c // DMA_EPOCH)
        self.ops.append(dict(eng=eng, fn=fn, deps=deps, dma=dma_key))
        return i

    def mm(self, out, lhsT, rhs, start=True, stop=True, **kw):
        self.op("pe", lambda e: e.matmul(out.ap, lhsT.ap, rhs.ap, start=start, stop=stop, **kw),
                reads=[lhsT, rhs], writes=[out])

    def transpose(self, out, in_, ident):
        self.op("pe", lambda e: e.transpose(out.ap, in_.ap, ident.ap), reads=[in_, ident], writes=[out])

    def act(self, out, in_, func, bias=None, scale=1.0, accum_out=None, eng="act", extra_reads=()):
        reads = [in_] + list(extra_reads)
        kw = {}
        if bias is not None:
            if isinstance(bias, Ref):
                reads.append(bias)
                kw["bias"] = bias.ap
            else:
                kw["bias"] = bias
        if isinstance(scale, Ref):
            reads.append(scale)
            kw["scale"] = scale.ap
        else:
            kw["scale"] = scale
        writes = [out]
        if accum_out is not None:
            writes.append(accum_out)
            kw["accum_out"] = accum_out.ap
        self.op(eng, lambda e: e.activation(out.ap, in_.ap, func, **kw), reads=reads, writes=writes)

    def tt(self, out, in0, in1, op, eng="dve"):
        self.op(eng, lambda e: e.tensor_tensor(out.ap, in0.ap, in1.ap, op), reads=[in0, in1], writes=[out])

    def ts(self, out, in0, s1, s2=None, op0=ALU.mult, op1=None, eng="dve", accum_out=None):
        reads = [in0]
        a1 = s1
        if isinstance(s1, Ref):
            reads.append(s1)
            a1 = s1.ap
        a2 = s2
        if isinstance(s2, Ref):
            reads.append(s2)
            a2 = s2.ap
        writes = [out]
        kw = {}
        if accum_out is not None:
            writes.append(accum_out)
            kw["accum_out"] = accum_out.ap
        if op1 is None:
            self.op(eng, lambda e: e.tensor_scalar(out.ap, in0.ap, a1, None, op0, **kw), reads=reads, writes=writes)
        else:
            self.op(eng, lambda e: e.tensor_scalar(out.ap, in0.ap, a1, a2, op0, op1, **kw), reads=reads, writes=writes)

    def stt(self, out, in0, scalar, in1, op0, op1, eng="dve"):
        reads = [in0, in1]
        a = scalar
        if isinstance(scalar, Ref):
            reads.append(scalar)
            a = scalar.ap
        self.op(eng, lambda e: e.scalar_tensor_tensor(out.ap, in0.ap, a, in1.ap, op0, op1), reads=reads, writes=[out])

    def copy(self, out, in_, eng="dve"):
        if eng == "act":
            self.op(eng, lambda e: e.copy(out.ap, in_.ap), reads=[in_], writes=[out])
        else:
            self.op(eng, lambda e: e.tensor_copy(out.ap, in_.ap), reads=[in_], writes=[out])

    def reduce(self, out, in_, op=ALU.add, axis=AX.X, eng="dve"):
        self.op(eng, lambda e: e.tensor_reduce(out.ap, in_.ap, axis, op), reads=[in_], writes=[out])

    def memset(self, out, val, eng="dve"):
        self.op(eng, lambda e: e.memset(out.ap, val), writes=[out])

    def recip(self, out, in_):
        self.op("dve", lambda e: e.reciprocal(out.ap, in_.ap), reads=[in_], writes=[out])

    def dma(self, out, in_, key="d", q="sp", **kw):
        self.op(q, lambda e: e.dma_start(out=out.ap, in_=in_.ap, **kw), reads=[in_], writes=[out], dma_key=key)

    def build(self):
        nc = self.nc
        ops = self.ops
        n = len(ops)
        signal = [False] * n
        for i, o in enumerate(ops):
            for d in o["deps"]:
                od = ops[d]
                if od["dma"] is None:
                    if od["eng"] == "pe" and o["eng"] == "pe" and o["dma"] is None:
                        continue
                    if (not SAME_ENGINE_SYNC) and od["eng"] == o["eng"] and o["dma"] is None:
                        continue
                    signal[d] = True
        ecnt = {e: 0 for e in self.ENG}
        sigval = [None] * n
        dkeys = {}
        dma_ids = {}
        for i, o in enumerate(ops):
            if o["dma"] is not None:
                dma_ids.setdefault(o["dma"], []).append(i)
            elif signal[i]:
                ecnt[o["eng"]] += 1
                c = ecnt[o["eng"]] - 1
                sigval[i] = (c // SEM_EPOCH, c % SEM_EPOCH + 1)
        import bisect
        stack = []
        esem = {}
        for e in self.ENG:
            for ep in range(max(1, (ecnt[e] + SEM_EPOCH - 1) // SEM_EPOCH)):
                g = nc.semaphore("sem_%s_%d" % (e, ep))
                esem[(e, ep)] = g.__enter__()
                stack.append(g)
        dsem = {}
        for k in dma_ids:
            g = nc.semaphore("dsem_%d" % len(dsem))
            dsem[k] = g.__enter__()
            stack.append(g)
        per_eng = {e: [] for e in self.ENG}
        for i, o in enumerate(ops):
            per_eng[o["eng"]].append(i)
        engobj = {"pe": "tensor", "act": "scalar", "dve": "vector", "pool": "gpsimd", "sp": "sync"}
        self.max_sem = dict(ecnt)

        def emit(ename, e):
            waited = {}
            for i in per_eng[ename]:
                o = ops[i]
                need = {}
                for d in o["deps"]:
                    od = ops[d]
                    if od["dma"] is not None:
                        k = od["dma"]
                        lst = dma_ids[k]
                        cnt = bisect.bisect_left(lst, i)
                        need[("d", k)] = max(need.get(("d", k), 0), 16 * cnt)
                    else:
                        if not signal[d]:
                            continue
                        if od["eng"] == "pe" and ename == "pe" and o["dma"] is None:
                            continue
                        if (not SAME_ENGINE_SYNC) and od["eng"] == ename and o["dma"] is None:
                            continue
                        key = ("e", od["eng"], sigval[d][0])
                        need[key] = max(need.get(key, 0), sigval[d][1])
                for key, v in need.items():
                    if waited.get(key, 0) >= v:
                        continue
                    waited[key] = v
                    s = dsem[key[1]] if key[0] == "d" else esem[(key[1], key[2])]
                    e.wait_ge(s, v)
                ins = o["fn"](e)
                if o["dma"] is not None:
                    ins.then_inc(dsem[o["dma"]], 16)
                elif signal[i]:
                    ins.then_inc(esem[(ename, sigval[i][0])], 1)
            if ename == "sp":
                for k, lst in dma_ids.items():
                    e.wait_ge(dsem[k], 16 * len(lst))

        with nc.Block() as block:
            @block.tensor
            def _(e):
                emit("pe", e)

            @block.scalar
            def _(e):
                emit("act", e)

            @block.vector
            def _(e):
                emit("dve", e)

            @block.gpsimd
            def _(e):
                emit("pool", e)

            @block.sync
            def _(e):
                emit("sp", e)
        for g in reversed(stack):
            g.__exit__(None, None, None)
        for g in reversed(self.ctxs):
            g.__exit__(None, None, None)
        return nc


def run(prog, in_maps, n=8):
    nc = prog.build()
    res = run_bass_kernel_spmd(nc, in_maps, core_ids=list(range(n)))
    return res.results


import math


def R(t, ap):
    return Ref(ap, t.key)


def qblocks(T, CTX):
    out = []
    q = 0
    while q < CTX:
        n = min(512, CTX - q)
        out.append((q, n, CTX // 128))
        q += n
    while q < T:
        n = min(512, T - q)
        out.append((q, n, T // 128))
        q += n
    return out


def attn_core(P, C, QT, KT, V, O, n_maps, dqk, slices_of_map, oslices_of_map, scale, identf):
    T, CTX = C["T"], C["CTX"]
    NT = T // 128
    nsl = len(slices_of_map(0))
    P.push()
    kt = [P.tile([dqk, T], BF16, "kt") for _ in range(2)]
    vt = [P.tile([128, NT, nsl, 65], BF16, "vt") for _ in range(2)]
    qt = [P.tile([dqk, 512], BF16, "qt") for _ in range(2)]
    sp = [P.psum([128, 2, 512], F32, "sp") for _ in range(2)]
    pt = [P.tile([128, 2, 512], BF16, "pt") for _ in range(3)]
    ops = P.psum([65, nsl, 512], F32, "ops")
    osb = [P.tile([65, nsl, 512], F32, "osb") for _ in range(2)]
    tp = P.psum([128, 4, 65], F32, "tp")
    otok = [P.tile([128, 4, 65], F32, "otok") for _ in range(2)]
    blocks = qblocks(T, CTX)
    cnt = 0
    evc = [0]
    pending = [None]
    for m in range(n_maps):
        sl = slices_of_map(m)
        osl = oslices_of_map(m)
        k_ = kt[m % 2]
        v_ = vt[m % 2]
        P.dma(k_[:], KT[m, 0:dqk, :], key="kt%d" % (m % 2))
        P.dma(v_[:], R(V, V.h[:, sl[0]:sl[0] + nsl, :].rearrange("(n p) s e -> p n s e", p=128)), key="vt%d" % (m % 2), q="pool")
        for bi, (q0, nq, nkt) in enumerate(blocks):
            q_ = qt[bi % 2]
            P.dma(q_[:, :nq], QT[m, 0:dqk, q0:q0 + nq], key="qt%d" % (bi % 2))
            iters = [(k2, min(2, nkt - k2)) for k2 in range(0, nkt, 2)]

            def emit_S(ii):
                k2, nk = iters[ii]
                s_ = sp[(cnt + ii) % 2]
                for j in range(nk):
                    P.mm(s_[:, j, :nq], k_[:, (k2 + j) * 128:(k2 + j + 1) * 128], q_[:, :nq])
            emit_S(0)
            if pending[0] is not None:
                pending[0]()
                pending[0] = None
            for ii, (k2, nk) in enumerate(iters):
                s_ = sp[(cnt + ii) % 2]
                p_ = pt[(cnt + ii) % 3]
                if ii + 1 < len(iters):
                    emit_S(ii + 1)
                P.act(p_[:, :nk, :nq], s_[:, :nk, :nq], AF.Exp, scale=scale)
                for j in range(nk):
                    for s in range(nsl):
                        P.mm(ops[:, s, :nq], v_[:, k2 + j, s, :], p_[:, j, :nq],
                             start=(k2 + j == 0), stop=(k2 + j == nkt - 1))
            cnt += len(iters)
            ob = osb[bi % 2]
            P.copy(ob[:, :, :nq], ops[:, :, :nq], eng="dve")

            def epilogue(ob=ob, nq=nq, q0=q0, osl=osl):
                for s in range(nsl):
                    ot = otok[evc[0] % 2]
                    n4 = nq // 128
                    for t4 in range(n4):
                        P.transpose(tp[:, t4, :], ob[:, s, t4 * 128:(t4 + 1) * 128], identf[:65, :65])
                    P.copy(ot[:, :n4, :], tp[:, :n4, :], eng="act")
                    P.dma(R(O, O.h[q0:q0 + nq, osl[s], :].rearrange("(n p) e -> p n e", p=128)), ot[:, :n4, :], key="ot%d" % (evc[0] % 2), q="pool")
                    evc[0] += 1
            pending[0] = epilogue
    if pending[0] is not None:
        pending[0]()
    P.pop()


EPS = 1e-6


def dview(t, ap):
    return Ref(ap, t.key)


class Net:
    def __init__(self, P, C):
        self.P = P
        self.C = C
        T = C["T"]
        self.NT = T // 128
        self.nctx = C["CTX"] // 128
        self.identf = P.tile([128, 128], F32, "identf")
        self.identb = P.tile([128, 128], BF16, "identb")
        idd = P.din("ident", [128, 128])
        P.dma(self.identf[:], idd[:], key="c")
        P.copy(self.identb[:], self.identf[:])
        self.X = P.dscratch("X", [T, 1024], F32)
        self.H2 = P.dscratch("H2", [T, 1024], BF16)
        self.AFF = P.dscratch("AFF", [T, 16], F32)
        self.MOD = P.dscratch("MOD", [2, 6, 128, 1024], F32)
        self.MOE = P.dscratch("MOE", [T, 1024], F32)
        self.QT = P.dscratch("QT", [16, 96, T], BF16)
        self.KT = P.dscratch("KT", [16, 96, T], BF16)
        self.V = P.dscratch("V", [T, 16, 65], BF16)
        self.O = P.dscratch("O", [T, 32, 65], F32)
        self.AFFT = P.dscratch("AFFT", [16, T], F32)
        self.xin = P.din("xall", [T, 1024])
        self.cT = P.din("cT", [128, 8, 2])
        self.out = P.dout("out", [C["SEQ"], 1024])
        self.epsb = P.tile([128, 1], F32, "epsb")
        self.POSF = P.tile([128, self.NT, 16], F32, "POSF")
        self.RH = P.tile([128, self.NT, 16, 5], BF16, "RH")
        self.idxt = [P.tile([128, 16], I32, "idx") for _ in range(2)]
        self.xst = [P.tile([128, 1024], BF16, "xs") for _ in range(2)]
        self.ygt = [P.tile([128, 1024], F32, "yg") for _ in range(2)]
        P.memset(self.epsb[:], EPS)

    def rstd(self, out, ss, n):
        P = self.P
        P.ts(out, ss, 1.0 / n, EPS, ALU.mult, ALU.add)
        P.act(out, out, AF.Sqrt)
        P.recip(out, out)

    def load_w(self, dst, src_ap, nk, N, stg, cnt, q="sp", key="w"):
        P = self.P
        for k in range(nk):
            P.dma(dst[:, k, :], Ref(src_ap[k * 128:(k + 1) * 128, :], "W"), key="%s%d" % (key, k % 2), q="pool")

    def transposeT(self, dstT, src, nblk, pT, eng="act"):
        P = self.P
        for b0 in range(0, nblk, 8):
            nb = min(8, nblk - b0)
            for j in range(nb):
                P.transpose(pT[:, j, :], src[:, (b0 + j) * 128:(b0 + j + 1) * 128], self.identb[:])
            P.copy(dstT[:, b0:b0 + nb, :], pT[:, :nb, :], eng=eng)

    def front(self, xt, A, B, sq, small, tmp, hb):
        P = self.P
        ss, rs = small
        P.act(sq[:], xt[:], AF.Square, accum_out=ss[:])
        self.rstd(rs[:], ss[:], 1024)
        P.stt(tmp[:], xt[:], rs[:], A, ALU.mult, ALU.mult)
        P.tt(hb[:], tmp[:], B, ALU.add, eng="pool")

    def mod_phase(self, l):
        P, C = self.P, self.C
        P.push()
        adaw = P.din("ada_w%d" % l, [1024, 6144])
        adab = P.din("ada_b%d" % l, [6144])
        ng = P.din("ng%d" % l, [2, 1024])
        ct = P.tile([128, 8, 2], F32, "ct")
        P.dma(ct[:], self.cT[:], key="m0")
        st = P.tile([128, 8, 2], F32, "st")
        P.act(st[:], ct[:], AF.Silu)
        sbc = P.tile([128, 8, 2, 128], F32, "sbc")
        P.copy(sbc[:], R(st, st.h[:].unsqueeze(3).broadcast_to([128, 8, 2, 128])))
        wb = [P.tile([128, 8, 512], F32, "wb") for _ in range(2)]
        bb = [P.tile([128, 512], F32, "bb") for _ in range(2)]
        gb = [P.tile([128, 512], F32, "gb") for _ in range(2)]
        pm = [P.psum([128, 512], F32, "pm") for _ in range(2)]
        res = [P.tile([128, 512], F32, "res") for _ in range(2)]
        slot_of = {0: 1, 1: 0, 2: 2, 3: 4, 4: 3, 5: 5}
        it = 0
        for blk in range(12):
            chunk = blk // 2
            c0 = (blk % 2) * 512
            w_ = wb[blk % 2]
            P.dma(w_[:], R(adaw, adaw.h[:, blk * 512:(blk + 1) * 512].rearrange("(k p) n -> p k n", p=128)), key="mw%d" % (blk % 2))
            b_ = bb[blk % 2]
            P.dma(b_[:], R(adab, adab.h[blk * 512:(blk + 1) * 512].partition_broadcast(128)), key="mb%d" % (blk % 2), q="pool")
            g_ = gb[blk % 2]
            if chunk in (1, 4):
                P.dma(g_[:], R(ng, ng.h[0 if chunk == 1 else 1, c0:c0 + 512].partition_broadcast(128)), key="mg%d" % (blk % 2), q="pool")
            for which in range(2):
                p_ = pm[it % 2]
                r_ = res[it % 2]
                it += 1
                cj = 1 if which == 0 else 0
                for k in range(8):
                    P.mm(p_[:], sbc[:, k, cj, :], w_[:, k, :], start=(k == 0), stop=(k == 7))
                P.tt(r_[:], p_[:], b_[:], ALU.add)
                if chunk in (1, 4):
                    P.stt(r_[:], r_[:], 1.0, g_[:], ALU.add, ALU.mult)
                P.dma(self.MOD[which, slot_of[chunk], :, c0:c0 + 512], r_[:], key="mo%d" % (it % 2), q="pool")
        P.pop()

    def load_mod(self, slots):
        P = self.P
        d = {}
        for which in range(2):
            for s in slots:
                t = P.tile([128, 1024], F32, "mod")
                P.dma(t[:], self.MOD[which, s], key="lm", q="pool")
                d[(which, s)] = t
        return d

    def xsrc(self, l):
        import os
        return self.xin if l == int(os.environ.get("MK_L0", "0")) else self.X

    def da_proj(self, l, j):
        P, C = self.P, self.C
        T, NT = C["T"], self.NT
        P.push()
        win = P.din("da_win%d" % j, [1024, 3072])
        qg = P.din("da_qg%d" % j, [64])
        kg = P.din("da_kg%d" % j, [64])
        cosd = P.din("cosD", [T, 32]) if "cosD" not in self.__dict__ else self.cosD
        self.cosD = cosd
        sind = P.din("sinD", [T, 32]) if "sinD" not in self.__dict__ else self.sinD
        self.sinD = sind
        w = P.tile([128, 8, 3072], BF16, "win")
        stg = [P.tile([128, 2048], F32, "stg") for _ in range(2)]
        self.load_w(w, win.h, 8, 3072, stg, [0])
        md = self.load_mod([0, 1])
        qkg = P.tile([128, 32, 64], F32, "qkg")
        P.dma(qkg[:, 0:16, :], R(qg, qg.h[:].partition_broadcast(128).unsqueeze(1).broadcast_to([128, 16, 64])), key="g0", q="pool")
        P.dma(qkg[:, 16:32, :], R(kg, kg.h[:].partition_broadcast(128).unsqueeze(1).broadcast_to([128, 16, 64])), key="g0", q="pool")
        xt = [P.tile([128, 1024], F32, "xt") for _ in range(2)]
        cs = [P.tile([128, 2, 32], F32, "cs") for _ in range(2)]
        sq = P.tile([128, 2048], F32, "sq")
        tmp = P.tile([128, 1024], F32, "tmp")
        hb = P.tile([128, 1024], BF16, "hb")
        hT = [P.tile([128, 8, 128], BF16, "hT") for _ in range(2)]
        small = [(P.tile([128, 1], F32, "ss"), P.tile([128, 1], F32, "rs")) for _ in range(2)]
        ss32 = P.tile([128, 32], F32, "ss32")
        rs32 = P.tile([128, 32], F32, "rs32")
        qn = P.tile([128, 32, 64], F32, "qn")
        t1 = P.tile([128, 32, 32], F32, "t1")
        t2 = P.tile([128, 32, 32], F32, "t2")
        t3 = P.tile([128, 32, 32], F32, "t3")
        t4 = P.tile([128, 32, 32], F32, "t4")
        qb = P.tile([128, 32, 64], BF16, "qb")
        qkT = [P.tile([128, 16, 128], BF16, "qkT") for _ in range(2)]
        va = [P.tile([128, 16, 65], BF16, "va") for _ in range(2)]
        for v_ in va:
            P.memset(v_[:], 1.0)
        pT = [P.psum([128, 8, 128], BF16, "pT") for _ in range(2)]
        pqk = P.psum([128, 4, 512], F32, "pqk")
        pv = P.psum([128, 2, 512], F32, "pv")
        X = self.xsrc(l)
        for i in range(NT):
            which = 0 if i < self.nctx else 1
            x_ = xt[i % 2]
            P.dma(x_[:], X[i * 128:(i + 1) * 128, :], key="x%d" % (i % 2))
            c_ = cs[i % 2]
            P.dma(c_[:, 0, :], cosd[i * 128:(i + 1) * 128, :], key="cs%d" % (i % 2), q="pool")
            P.dma(c_[:, 1, :], sind[i * 128:(i + 1) * 128, :], key="cs%d" % (i % 2), q="pool")
            self.front(x_, md[(which, 0)][:], md[(which, 1)][:], R(sq, sq.h[:, :1024]), small[i % 2], tmp, hb)
            h_ = hT[i % 2]
            self.transposeT(h_, hb, 8, pT[0])
            for n in range(4):
                for k in range(8):
                    P.mm(pqk[:, n, :], h_[:, k, :], w[:, k, n * 512:(n + 1) * 512], start=(k == 0), stop=(k == 7))
            for n in range(2):
                for k in range(8):
                    P.mm(pv[:, n, :], h_[:, k, :], w[:, k, 2048 + n * 512:2048 + (n + 1) * 512], start=(k == 0), stop=(k == 7))
            pq3 = R(pqk, pqk.h[:].rearrange("p n (h d) -> p (n h) d", d=64))
            P.act(sq[:], R(pqk, pqk.h[:].rearrange("p n f -> p (n f)")), AF.Square)
            P.reduce(ss32[:], R(sq, sq.h[:].rearrange("p (h d) -> p h d", d=64)))
            self.rstd(rs32[:], ss32[:], 64)
            P.tt(qn[:], pq3, R(rs32, rs32.h[:].unsqueeze(2).broadcast_to([128, 32, 64])), ALU.mult)
            P.tt(qn[:], qn[:], qkg[:], ALU.mult, eng="pool")
            cosb = R(c_, c_.h[:, 0, :].unsqueeze(1).broadcast_to([128, 32, 32]))
            sinb = R(c_, c_.h[:, 1, :].unsqueeze(1).broadcast_to([128, 32, 32]))
            x1 = qn[:, :, 0:32]
            x2 = qn[:, :, 32:64]
            P.tt(t1[:], x1, cosb, ALU.mult)
            P.tt(t2[:], x2, sinb, ALU.mult, eng="pool")
            P.tt(qb[:, :, 0:32], t1[:], t2[:], ALU.subtract)
            P.tt(t3[:], x2, cosb, ALU.mult, eng="pool")
            P.tt(t4[:], x1, sinb, ALU.mult)
            P.tt(qb[:, :, 32:64], t3[:], t4[:], ALU.add, eng="pool")
            q_ = qkT[i % 2]
            self.transposeT(q_, R(qb, qb.h[:].rearrange("p h d -> p (h d)")), 16, pT[1], eng="dve")
            for two in range(2):
                P.dma(R(self.QT, self.QT.h[:, 0:64, i * 128:(i + 1) * 128].rearrange("(j two) d t -> two d j t", two=2)[two]), q_[two * 64:(two + 1) * 64, 0:8, :], key="qo%d" % (i % 2))
                P.dma(R(self.KT, self.KT.h[:, 0:64, i * 128:(i + 1) * 128].rearrange("(j two) d t -> two d j t", two=2)[two]), q_[two * 64:(two + 1) * 64, 8:16, :], key="qo%d" % (i % 2))
            v_ = va[i % 2]
            P.copy(v_[:, :, 0:64], R(pv, pv.h[:].rearrange("p n (h d) -> p (n h) d", d=64)), eng="act")
            P.dma(self.V[i * 128:(i + 1) * 128, :, :], v_[:], key="vo%d" % (i % 2), q="pool")
        P.pop()

    def post_alloc(self, l, wout_name):
        P = self.P
        wo_d = P.din(wout_name, [1024, 1024])
        rt_d = P.din("rt%d" % l, [1024, 16])
        S = {}
        S["wo"] = P.tile([128, 8, 1024], BF16, "wo")
        stg = [P.tile([128, 2048], F32, "stg") for _ in range(2)]
        self.load_w(S["wo"], wo_d.h, 8, 1024, stg, [0])
        rtf = P.tile([128, 8, 16], F32, "rtf")
        P.dma(rtf[:], R(rt_d, rt_d.h.rearrange("(k p) n -> p k n", p=128)), key="rt")
        S["rt"] = P.tile([128, 8, 16], BF16, "rtb")
        P.copy(S["rt"][:], rtf[:])
        S["md"] = self.load_mod([2, 3, 4])
        S["oT"] = [P.tile([128, 8, 128], BF16, "oT") for _ in range(2)]
        S["xt"] = [P.tile([128, 1024], F32, "xt") for _ in range(2)]
        S["xn"] = [P.tile([128, 1024], F32, "xn") for _ in range(2)]
        S["sq"] = P.tile([128, 1024], F32, "sq")
        S["tmp"] = P.tile([128, 1024], F32, "tmp")
        S["h2"] = [P.tile([128, 1024], BF16, "h2") for _ in range(2)]
        S["h2T"] = [P.tile([128, 8, 128], BF16, "h2T") for _ in range(2)]
        S["small"] = [(P.tile([128, 1], F32, "ss"), P.tile([128, 1], F32, "rs")) for _ in range(2)]
        S["sm"] = [[P.tile([128, 1], F32, "sm") for _ in range(3)] for _ in range(2)]
        S["e"] = [P.tile([128, 16], F32, "e") for _ in range(2)]
        S["aff"] = [P.tile([128, 16], F32, "aff") for _ in range(2)]
        S["pT"] = P.psum([128, 8, 128], BF16, "pT")
        S["py"] = P.psum([128, 2, 512], F32, "py")
        S["pr"] = P.psum([128, 16], F32, "pr")
        S["pa"] = P.psum([16, 128], F32, "pa")
        S["at"] = [P.tile([16, 128], F32, "at") for _ in range(2)]
        return S

    def post(self, l, S, i, ob):
        P = self.P
        which = 0 if i < self.nctx else 1
        md = S["md"]
        oT = S["oT"][i % 2]
        self.transposeT(oT, ob, 8, S["pT"])
        py = S["py"]
        for n in range(2):
            for k in range(8):
                P.mm(py[:, n, :], oT[:, k, :], S["wo"][:, k, n * 512:(n + 1) * 512], start=(k == 0), stop=(k == 7))
        x_ = S["xt"][i % 2]
        X = self.xsrc(l)
        P.dma(x_[:], X[i * 128:(i + 1) * 128, :], key="px%d" % (i % 2))
        xn = S["xn"][i % 2]
        P.tt(xn[:], R(py, py.h[:].rearrange("p n f -> p (n f)")), md[(which, 2)][:], ALU.mult)
        P.tt(xn[:], xn[:], x_[:], ALU.add, eng="pool")
        P.dma(self.X[i * 128:(i + 1) * 128, :], xn[:], key="pxo%d" % (i % 2), q="pool")
        h2 = S["h2"][i % 2]
        self.front(xn, md[(which, 3)][:], md[(which, 4)][:], S["sq"], S["small"][i % 2], S["tmp"], h2)
        P.dma(self.H2[i * 128:(i + 1) * 128, :], h2[:], key="ph%d" % (i % 2), q="pool")
        h2T = S["h2T"][i % 2]
        self.transposeT(h2T, h2, 8, S["pT"])
        pr = S["pr"]
        for k in range(8):
            P.mm(pr[:], h2T[:, k, :], S["rt"][:, k, :], start=(k == 0), stop=(k == 7))
        mx, sm, rc = S["sm"][i % 2]
        P.reduce(mx[:], pr[:], op=ALU.max)
        P.ts(mx[:], mx[:], -1.0, None, ALU.mult)
        e = S["e"][i % 2]
        P.act(e[:], pr[:], AF.Exp, bias=mx[:], scale=1.0, accum_out=sm[:])
        P.recip(rc[:], sm[:])
        aff = S["aff"][i % 2]
        P.ts(aff[:], e[:], rc[:], None, ALU.mult)
        P.dma(self.AFF[i * 128:(i + 1) * 128, :], aff[:], key="pa%d" % (i % 2), q="pool")
        P.transpose(S["pa"][:], aff[:], self.identf[:])
        at = S["at"][i % 2]
        P.copy(at[:], S["pa"][:], eng="act")
        P.dma(self.AFFT[:, i * 128:(i + 1) * 128], at[:], key="pat%d" % (i % 2), q="pool")

    def da_finish(self, l, j):
        P, C = self.P, self.C
        NT = self.NT
        lam_init = 0.8 - 0.6 * math.exp(-0.3 * l)
        P.push()
        S = self.post_alloc(l, "da_wout%d" % j)
        lamd = P.din("da_lam%d" % j, [256])
        sgd = P.din("da_sg%d" % j, [128])
        lt = P.tile([128, 256], F32, "lt")
        P.dma(lt[:], R(lamd, lamd.h[:].partition_broadcast(128)), key="fl")
        pr2 = P.tile([128, 2, 64], F32, "pr2")
        P.tt(pr2[:, 0, :], lt[:, 0:64], lt[:, 64:128], ALU.mult)
        P.tt(pr2[:, 1, :], lt[:, 128:192], lt[:, 192:256], ALU.mult)
        s2 = P.tile([128, 2], F32, "s2")
        P.reduce(s2[:], pr2[:])
        e2 = P.tile([128, 2], F32, "e2")
        P.act(e2[:], s2[:], AF.Exp)
        nl = P.tile([128, 1], F32, "nl")
        P.tt(nl[:], e2[:, 1:2], e2[:, 0:1], ALU.subtract)
        P.ts(nl[:], nl[:], -lam_init, None, ALU.add)
        sg = P.tile([128, 8, 128], F32, "sg")
        P.dma(sg[:], R(sgd, sgd.h[:].partition_broadcast(128).unsqueeze(1).broadcast_to([128, 8, 128])), key="fl")
        P.ts(sg[:], sg[:], 1.0 - lam_init, None, ALU.mult)
        ot = [P.tile([128, 32, 65], F32, "ot") for _ in range(2)]
        rd = P.tile([128, 32], F32, "rd")
        on = P.tile([128, 32, 64], F32, "on")
        df = P.tile([128, 8, 128], F32, "df")
        sq = P.tile([128, 8, 128], F32, "sqd")
        ss8 = P.tile([128, 8], F32, "ss8")
        rs8 = P.tile([128, 8], F32, "rs8")
        ob = [P.tile([128, 1024], BF16, "ob") for _ in range(2)]
        for i in range(NT):
            o_ = ot[i % 2]
            P.dma(o_[:], self.O[i * 128:(i + 1) * 128, :, :], key="fo%d" % (i % 2))
            P.recip(rd[:], o_[:, :, 64])
            P.tt(on[:], o_[:, :, 0:64], R(rd, rd.h[:].unsqueeze(2).broadcast_to([128, 32, 64])), ALU.mult)
            on4 = on.h[:].rearrange("p (h m s) d -> p h m (s d)", h=8, m=2)
            P.stt(df[:], R(on, on4[:, :, 1, :]), nl[:], R(on, on4[:, :, 0, :]), ALU.mult, ALU.add)
            P.act(sq[:], df[:], AF.Square)
            P.reduce(ss8[:], sq[:])
            self.rstd(rs8[:], ss8[:], 128)
            P.tt(df[:], df[:], R(rs8, rs8.h[:].unsqueeze(2).broadcast_to([128, 8, 128])), ALU.mult)
            o2 = ob[i % 2]
            P.tt(R(o2, o2.h[:].rearrange("p (h d) -> p h d", d=128)), df[:], sg[:], ALU.mult, eng="pool")
            self.post(l, S, i, o2)
        P.pop()

    def moe_phase(self, l, last):
        P, C = self.P, self.C
        T, NT, CTX, SEQ, FF, NE = C["T"], self.NT, C["CTX"], C["SEQ"], C["FF"], 16
        nctx = self.nctx
        NF = FF // 128
        sets = [(0, CTX, 2 * CTX // NE), (CTX, SEQ, 2 * SEQ // NE)]
        nrows = sum(NE * s_[2] for s_ in sets)
        POSF, RH = self.POSF, self.RH
        P.push()
        zt = P.tile([128, 1024], F32, "zt")
        P.memset(zt[:], 0.0)
        for i in range(NT):
            P.dma(self.MOE[i * 128:(i + 1) * 128, :], zt[:], key="mz", q="pool")
        maskT = P.tile([16, T], BF16, "maskT")
        lo = P.tile([16, 1], F32, "lo")
        hi = P.tile([16, 1], F32, "hi")
        mid = P.tile([16, 1], F32, "mid")
        cntt = P.tile([16, 1], F32, "cnt")
        mm_ = P.tile([16, 1], F32, "m")
        d1 = P.tile([16, 1], F32, "d1")
        d2 = P.tile([16, 1], F32, "d2")
        cmp = P.tile([16, max(CTX, SEQ)], F32, "cmp")
        affT = P.tile([16, T], F32, "affT")
        P.dma(affT[:], self.AFFT[:], key="mza")
        for (t0, n, cap) in sets:
            a_ = affT[:, t0:t0 + n]
            P.memset(lo[:], 0.0)
            P.memset(hi[:], 1.0001)
            for it in range(34):
                P.tt(mid[:], lo[:], hi[:], ALU.add)
                P.ts(mid[:], mid[:], 0.5, None, ALU.mult)
                P.ts(cmp[:, :n], a_, mid[:], None, ALU.is_ge)
                P.reduce(cntt[:], cmp[:, :n])
                P.ts(mm_[:], cntt[:], float(cap) - 0.5, None, ALU.is_ge)
                P.tt(d1[:], mid[:], lo[:], ALU.subtract)
                P.tt(d2[:], hi[:], mid[:], ALU.subtract)
                P.stt(lo[:], d1[:], mm_[:], lo[:], ALU.mult, ALU.add)
                P.stt(hi[:], d2[:], mm_[:], mid[:], ALU.mult, ALU.add)
            P.ts(maskT[:, t0:t0 + n], a_, lo[:], None, ALU.is_ge)
        U = P.tile([128, 128], BF16, "U")
        ones = P.tile([128, 128], BF16, "ones")
        Ud = P.din("U", [128, 128])
        uf = P.tile([128, 128], F32, "uf")
        P.dma(uf[:], Ud[:], key="cu")
        P.copy(U[:], uf[:])
        P.memset(ones[:], 1.0)
        tokd = P.din("tokidx", [128, NT])
        tokt = P.tile([128, NT], F32, "tokt")
        P.dma(tokt[:], tokd[:], key="cu")
        ecap = P.tile([128, 2, 16], F32, "ecap")
        ecd = P.din("ecap", [128, 2, 16])
        P.dma(ecap[:], ecd[:], key="cu")
        dumpd = P.din("dump", [128, 1])
        dump = P.tile([128, 1], F32, "dump")
        P.dma(dump[:], dumpd[:], key="cu")
        cum = P.tile([128, 16], BF16, "cum")
        cumf = P.tile([128, 16], F32, "cumf")
        pm = P.psum([128, 16], BF16, "pmk")
        pp = P.psum([128, 16], F32, "ppos")
        mt = [P.tile([128, 16], BF16, "mt") for _ in range(2)]
        mtf = [P.tile([128, 16], F32, "mtf") for _ in range(2)]
        pos = [P.tile([128, 16], F32, "pos") for _ in range(2)]
        ok = [P.tile([128, 16], F32, "ok") for _ in range(2)]
        afft = [P.tile([128, 16], F32, "afft") for _ in range(2)]
        gr1 = P.tile([128, 16], F32, "gr1")
        gr2 = P.tile([128, 16], F32, "gr2")
        ipcd = P.din("ipc", [128, NT, 2])
        ipc = P.tile([128, NT, 2], F32, "ipc")
        P.dma(ipc[:], ipcd[:], key="cu")
        for si, (t0, n, cap) in enumerate(sets):
            P.memset(cumf[:], 0.0)
            P.copy(cum[:], cumf[:])
            for i in range(t0 // 128, (t0 + n) // 128):
                m_ = mt[i % 2]
                P.transpose(pm[:], maskT[:, i * 128:(i + 1) * 128], self.identb[:16, :16])
                P.copy(m_[:], pm[:])
                P.copy(mtf[i % 2][:], pm[:], eng="act")
                P.mm(pp[:], U[:], m_[:], start=True, stop=False)
                P.mm(pp[:], ones[:], cum[:], start=False, stop=True)
                p_ = pos[i % 2]
                P.copy(p_[:], pp[:])
                P.tt(cumf[:], cumf[:], mtf[i % 2][:], ALU.add)
                P.copy(cum[:], cumf[:])
                o_ = ok[i % 2]
                P.ts(o_[:], p_[:], float(cap) - 0.5, None, ALU.is_lt)
                P.tt(o_[:], o_[:], mtf[i % 2][:], ALU.mult)
                P.ts(p_[:], p_[:], 1.0, None, ALU.add)
                P.tt(p_[:], p_[:], o_[:], ALU.mult)
                P.ts(POSF[:, i, :], p_[:], -1.0, None, ALU.add)
                a_ = afft[i % 2]
                P.dma(a_[:], self.AFF[i * 128:(i + 1) * 128, :], key="ca%d" % (i % 2))
                P.copy(RH[:, i, :, 0:2], R(ipc, ipc.h[:, i, :].unsqueeze(1).broadcast_to([128, 16, 2])), eng="pool")
                P.copy(RH[:, i, :, 2], a_[:])
                P.tt(gr1[:], a_[:], RH[:, i, :, 2], ALU.subtract)
                P.copy(RH[:, i, :, 3], gr1[:])
                P.tt(gr2[:], gr1[:], RH[:, i, :, 3], ALU.subtract)
                P.copy(RH[:, i, :, 4], gr2[:])
        P.pop()

        P.push()
        wgd = P.din("wg%d" % l, [NE, 1024, FF])
        wud = P.din("wu%d" % l, [NE, 1024, FF])
        wdd = P.din("wd%d" % l, [NE, FF, 1024])
        GW = min(512, FF)
        NG = FF // GW
        F4 = GW // 128
        wgg = [P.tile([128, 8, GW], BF16, "wgg") for _ in range(2)]
        wug = [P.tile([128, 8, GW], BF16, "wug") for _ in range(2)]
        wd = P.tile([128, NF, 1024], BF16, "wd")
        cnt = [0]
        slot_tiles = []
        s0 = 0
        for si, (t0, n, cap) in enumerate(sets):
            for b0 in range(0, cap, 128):
                ns = min(128, cap - b0)
                slot_tiles.append((si, b0, s0, ns))
                s0 += ns
        NSLOT = s0
        NTS = len(slot_tiles)
        blocks = []
        b = 0
        for si, (t0, n, cap) in enumerate(sets):
            for b0 in range(0, cap, 512):
                nb = min(512, cap - b0)
                blocks.append((b, nb))
                b += nb
        xs = self.xst
        xsT = P.tile([128, 8, NSLOT], BF16, "xsT")
        actT = P.tile([128, NF, NSLOT], BF16, "actT")
        lst = [P.tile([128, NTS, 2], F32, "lst") for _ in range(2)]
        idx = self.idxt
        sg_ = [P.tile([128, 512], F32, "sgl") for _ in range(2)]
        yg = self.ygt
        pT = P.psum([128, 8, 128], BF16, "pT")
        pg = [P.psum([128, 512], F32, "pg") for _ in range(2)]
        pu = [P.psum([128, 512], F32, "pu") for _ in range(2)]
        py = P.psum([128, 2, 512], F32, "py")
        bases = []
        base = 0
        for (t0, n, cap) in sets:
            bases.append(base)
            base += NE * cap

        def stage_cast(dst_ref, src_ap, key):
            P.dma(dst_ref, Ref(src_ap, "W"), key=key, q="pool")

        def load_group(e, g):
            gi = e * NG + g
            for (dst, srcd) in ((wgg[gi % 2], wgd), (wug[gi % 2], wud)):
                src3 = srcd.h[e].rearrange("(k p) n -> p k n", p=128)
                for k0 in range(0, 8, 4):
                    stage_cast(dst[:, k0:k0 + 4, :], src3[:, k0:k0 + 4, g * GW:(g + 1) * GW], "wq%d" % (gi % 2))

        def load_wd(e):
            src3 = wdd.h[e].rearrange("(f p) n -> p f n", p=128)
            for f0 in range(0, NF, 2):
                stage_cast(wd[:, f0:f0 + 2, :], src3[:, f0:f0 + 2, :], "wdk")
        iotad = P.din("iota", [128, 1024])
        iota = P.tile([128, 1024], F32, "iota")
        P.dma(iota[:], iotad[:], key="cu")
        oh = [P.tile([128, 128], BF16, "oh") for _ in range(4)]
        pacc2 = P.psum([128, 2, 8], F32, "pacc")
        ohc = [0]
        pacs = [P.tile([128, 5], F32, "pacs") for _ in range(2)]

        def compact(e):
            l_ = lst[e % 2]
            P.memset(l_[:], 0.0)
            for ti, (si, b0, sl0, ns) in enumerate(slot_tiles):
                t0, n, cap = sets[si]
                tiles = list(range(t0 // 128, (t0 + n) // 128))
                pa = pacc2[:, ti % 2, :]
                for i in tiles:
                    o_ = oh[ohc[0] % 4]
                    ohc[0] += 1
                    P.ts(o_[:, :ns], iota[:, b0:b0 + ns], POSF[:, i, e:e + 1], None, ALU.is_equal)
                    P.mm(pa[:ns, 0:5], o_[:, :ns], RH[:, i, e, :], start=(i == tiles[0]), stop=(i == tiles[-1]))
                pc = pacs[ti % 2]
                P.copy(pc[:ns, :], pa[:ns, 0:5], eng="act")
                P.stt(l_[:ns, ti, 0:1], pc[:ns, 0:1], 128.0, pc[:ns, 1:2], ALU.mult, ALU.add)
                P.reduce(l_[:ns, ti, 1:2], pc[:ns, 2:5])
        compact(0)
        load_group(0, 0)
        for e in range(NE):
            l_ = lst[e % 2]
            i_ = idx[e % 2]
            P.copy(i_[:, :NTS], l_[:, :, 0])
            for ti, (si, b0, sl0, ns) in enumerate(slot_tiles):
                x_ = xs[ti % 2]
                a1, a2, a3 = x_.h[:ns, :], self.H2.h[:, :], i_.h[:ns, ti:ti + 1]
                P.op("pool", (lambda en, a1=a1, a2=a2, a3=a3: en.indirect_dma_start(
                    out=a1, out_offset=None, in_=a2, in_offset=bass.IndirectOffsetOnAxis(ap=a3, axis=0))),
                    reads=[self.H2[:], i_[:]], writes=[x_[:]], dma_key="fg%d" % (ti % 2))
                for k in range(8):
                    P.transpose(pT[:, k, :ns], x_[:ns, k * 128:(k + 1) * 128], self.identb[:ns, :ns])
                P.copy(xsT[:, :, sl0:sl0 + ns], pT[:, :, :ns], eng="act")
            if e + 1 < NE:
                compact(e + 1)
            for g in range(NG):
                if g + 1 < NG:
                    load_group(e, g + 1)
                elif e + 1 < NE:
                    load_group(e + 1, 0)
                if g == 0:
                    load_wd(e)
                wg_, wu_ = wgg[(e * NG + g) % 2], wug[(e * NG + g) % 2]
                for (bs, nb) in blocks:
                    for f4 in range(F4):
                        f = g * F4 + f4
                        g_ = pg[f % 2]
                        u_ = pu[f % 2]
                        for k in range(8):
                            P.mm(g_[:, :nb], wg_[:, k, f4 * 128:(f4 + 1) * 128], xsT[:, k, bs:bs + nb], start=(k == 0), stop=(k == 7))
                        for k in range(8):
                            P.mm(u_[:, :nb], wu_[:, k, f4 * 128:(f4 + 1) * 128], xsT[:, k, bs:bs + nb], start=(k == 0), stop=(k == 7))
                        s_ = sg_[f % 2]
                        P.act(s_[:, :nb], g_[:, :nb], AF.Silu)
                        P.tt(actT[:, f, bs:bs + nb], s_[:, :nb], u_[:, :nb], ALU.mult)
            for ti, (si, b0, sl0, ns) in enumerate(slot_tiles):
                for nn in range(2):
                    for f in range(NF):
                        P.mm(py[:ns, nn, :], actT[:, f, sl0:sl0 + ns], wd[:, f, nn * 512:(nn + 1) * 512], start=(f == 0), stop=(f == NF - 1))
                y_ = yg[ti % 2]
                P.ts(y_[:ns, :], R(py, py.h[:ns].rearrange("p n f -> p (n f)")), l_[:ns, ti, 1:2], None, ALU.mult)
                a1, a2, a3 = self.MOE.h[:, :], i_.h[:ns, ti:ti + 1], y_.h[:ns, :]
                P.op("pool", (lambda en, a1=a1, a2=a2, a3=a3: en.indirect_dma_start(
                    out=a1, out_offset=bass.IndirectOffsetOnAxis(ap=a2, axis=0), in_=a3, in_offset=None, compute_op=ALU.add)),
                    reads=[y_[:], i_[:], self.MOE[:]], writes=[self.MOE[:]], dma_key="fs")
        P.pop()
        P.push()
        md = self.load_mod([5])
        xt = [P.tile([128, 1024], F32, "xt") for _ in range(2)]
        mo = [P.tile([128, 1024], F32, "mo") for _ in range(2)]
        for i in range(NT):
            which = 0 if i < nctx else 1
            if last and which == 0:
                continue
            x_ = xt[i % 2]
            m_ = mo[i % 2]
            P.dma(x_[:], self.X[i * 128:(i + 1) * 128, :], key="cx%d" % (i % 2))
            P.dma(m_[:], self.MOE[i * 128:(i + 1) * 128, :], key="cm%d" % (i % 2), q="pool")
            P.tt(m_[:], m_[:], md[(which, 5)][:], ALU.mult)
            P.tt(x_[:], x_[:], m_[:], ALU.add, eng="pool")
            if last:
                j = i - nctx
                P.dma(self.out[j * 128:(j + 1) * 128, :], x_[:], key="co%d" % (i % 2))
            else:
                P.dma(self.X[i * 128:(i + 1) * 128, :], x_[:], key="co%d" % (i % 2))
        P.pop()


    def mla_layer(self, l, j):
        import os
        P, C = self.P, self.C
        T, NT = C["T"], self.NT
        P.push()
        wdn_d = P.din("mla_wdown%d" % j, [1024, 416])
        wuq_d = P.din("mla_wuq%d" % j, [256, 1536])
        wukv_d = P.din("mla_wukv%d" % j, [128, 2048])
        qag_d = P.din("mla_qag%d" % j, [256])
        kvag_d = P.din("mla_kvag%d" % j, [128])
        qg_d = P.din("mla_qg%d" % j, [96])
        kg_d = P.din("mla_kg%d" % j, [96])
        cosd = P.din("cosM", [T, 16])
        sind = P.din("sinM", [T, 16])
        stg = [P.tile([128, 2048], F32, "stg") for _ in range(2)]
        cnt = [0]
        wdn = P.tile([128, 8, 416], BF16, "wdn")
        self.load_w(wdn, wdn_d.h, 8, 416, stg, cnt)
        wuq = P.tile([128, 2, 1536], BF16, "wuq")
        self.load_w(wuq, wuq_d.h, 2, 1536, stg, cnt)
        wukv = P.tile([128, 1, 2048], BF16, "wukv")
        self.load_w(wukv, wukv_d.h, 1, 2048, stg, cnt)
        md = self.load_mod([0, 1])
        qag = P.tile([128, 256], F32, "qag")
        P.dma(qag[:], R(qag_d, qag_d.h[:].partition_broadcast(128)), key="g0", q="pool")
        kvag = P.tile([128, 128], F32, "kvag")
        P.dma(kvag[:], R(kvag_d, kvag_d.h[:].partition_broadcast(128)), key="g0", q="pool")
        qkg = P.tile([128, 32, 96], F32, "qkg")
        P.dma(qkg[:, 0:16, :], R(qg_d, qg_d.h[:].partition_broadcast(128).unsqueeze(1).broadcast_to([128, 16, 96])), key="g0", q="pool")
        P.dma(qkg[:, 16:32, :], R(kg_d, kg_d.h[:].partition_broadcast(128).unsqueeze(1).broadcast_to([128, 16, 96])), key="g0", q="pool")
        xt = [P.tile([128, 1024], F32, "xt") for _ in range(2)]
        cs = [P.tile([128, 2, 16], F32, "cs") for _ in range(2)]
        sq = P.tile([128, 32 * 96], F32, "sq")
        tmp = P.tile([128, 1024], F32, "tmp")
        hb = P.tile([128, 1024], BF16, "hb")
        hT = [P.tile([128, 8, 128], BF16, "hT") for _ in range(2)]
        small = [(P.tile([128, 1], F32, "ss"), P.tile([128, 1], F32, "rs")) for _ in range(2)]
        sa = [P.tile([128, 1], F32, "sa") for _ in range(4)]
        cqn = P.tile([128, 384], BF16, "cqn")
        cT_ = P.tile([128, 3, 128], BF16, "cT_")
        krs = P.tile([128, 32], F32, "krs")
        qk = P.tile([128, 32, 96], F32, "qk")
        ss32 = P.tile([128, 32], F32, "ss32")
        rs32 = P.tile([128, 32], F32, "rs32")
        t1 = P.tile([128, 32, 16], F32, "t1")
        t2 = P.tile([128, 32, 16], F32, "t2")
        t3 = P.tile([128, 32, 16], F32, "t3")
        t4 = P.tile([128, 32, 16], F32, "t4")
        qb = P.tile([128, 32, 96], BF16, "qb")
        qkT = [P.tile([96, 32, 128], BF16, "qkT") for _ in range(2)]
        va = [P.tile([128, 16, 65], BF16, "va") for _ in range(2)]
        for v_ in va:
            P.memset(v_[:], 1.0)
        pT = P.psum([128, 8, 128], BF16, "pT")
        plat = P.psum([128, 416], F32, "plat")
        pq = P.psum([128, 3, 512], F32, "pq")
        pkv = P.psum([128, 2, 512], F32, "pkv")
        X = self.xsrc(l)
        for i in range(NT):
            which = 0 if i < self.nctx else 1
            x_ = xt[i % 2]
            P.dma(x_[:], X[i * 128:(i + 1) * 128, :], key="x%d" % (i % 2))
            c_ = cs[i % 2]
            P.dma(c_[:, 0, :], cosd[i * 128:(i + 1) * 128, :], key="cs%d" % (i % 2), q="pool")
            P.dma(c_[:, 1, :], sind[i * 128:(i + 1) * 128, :], key="cs%d" % (i % 2), q="pool")
            self.front(x_, md[(which, 0)][:], md[(which, 1)][:], sq[:, :1024], small[i % 2], tmp, hb)
            h_ = hT[i % 2]
            self.transposeT(h_, hb, 8, pT)
            for k in range(8):
                P.mm(plat[:], h_[:, k, :], wdn[:, k, :], start=(k == 0), stop=(k == 7))
            P.act(sq[:, 0:256], plat[:, 0:256], AF.Square, accum_out=sa[0][:])
            self.rstd(sa[1][:], sa[0][:], 256)
            P.stt(cqn[:, 0:256], plat[:, 0:256], sa[1][:], qag[:], ALU.mult, ALU.mult)
            P.act(sq[:, 256:384], plat[:, 256:384], AF.Square, accum_out=sa[2][:])
            self.rstd(sa[3][:], sa[2][:], 128)
            P.stt(cqn[:, 256:384], plat[:, 256:384], sa[3][:], kvag[:], ALU.mult, ALU.mult)
            P.copy(krs[:], plat[:, 384:416], eng="act")
            STOP = float(os.environ.get("MK_STOP", "9"))
            if STOP <= 1:
                continue
            self.transposeT(cT_, cqn, 3, pT)
            if STOP <= 1.2:
                continue
            for n in range(3):
                for k in range(2):
                    P.mm(pq[:, n, :], cT_[:, k, :], wuq[:, k, n * 512:(n + 1) * 512], start=(k == 0), stop=(k == 1))
            qkflat = qk.h[:, 0:16, :].rearrange("p h d -> p (h d)")
            for n in range(3):
                P.copy(R(qk, qkflat[:, n * 512:(n + 1) * 512]), pq[:, n, :], eng=("act" if n % 2 == 0 else "dve"))
            if STOP <= 1.4:
                continue
            v_ = va[i % 2]
            for hh in range(2):
                for n in range(2):
                    P.mm(pkv[:, n, :], cT_[:, 2, :], wukv[:, 0, hh * 1024 + n * 512:hh * 1024 + (n + 1) * 512], start=True, stop=True)
                kv3 = pkv.h[:].rearrange("p n (h d) -> p (n h) d", d=128)
                P.copy(qk[:, 16 + hh * 8:16 + (hh + 1) * 8, 0:64], R(pkv, kv3[:, :, 0:64]))
                P.copy(v_[:, hh * 8:(hh + 1) * 8, 0:64], R(pkv, kv3[:, :, 64:128]), eng="act")
            if STOP <= 1.6:
                continue
            P.copy(qk[:, 16:32, 64:96], R(krs, krs.h[:].unsqueeze(1).broadcast_to([128, 16, 32])), eng="pool")
            if STOP <= 1.8:
                continue
            P.dma(self.V[i * 128:(i + 1) * 128, :, :], v_[:], key="vo%d" % (i % 2), q="pool")
            if STOP <= 2:
                continue
            P.act(R(sq, sq.h[:].rearrange("p (h d) -> p h d", d=96)), qk[:], AF.Square)
            P.reduce(ss32[:], R(sq, sq.h[:].rearrange("p (h d) -> p h d", d=96)))
            self.rstd(rs32[:], ss32[:], 96)
            P.tt(qk[:], qk[:], R(rs32, rs32.h[:].unsqueeze(2).broadcast_to([128, 32, 96])), ALU.mult)
            P.tt(qk[:], qk[:], qkg[:], ALU.mult, eng="pool")
            P.copy(qb[:, :, 0:64], qk[:, :, 0:64], eng="act")
            cosb = R(c_, c_.h[:, 0, :].unsqueeze(1).broadcast_to([128, 32, 16]))
            sinb = R(c_, c_.h[:, 1, :].unsqueeze(1).broadcast_to([128, 32, 16]))
            x1 = qk[:, :, 64:80]
            x2 = qk[:, :, 80:96]
            P.tt(t1[:], x1, cosb, ALU.mult)
            P.tt(t2[:], x2, sinb, ALU.mult, eng="pool")
            P.tt(qb[:, :, 64:80], t1[:], t2[:], ALU.subtract)
            P.tt(t3[:], x2, cosb, ALU.mult, eng="pool")
            P.tt(t4[:], x1, sinb, ALU.mult)
            P.tt(qb[:, :, 80:96], t3[:], t4[:], ALU.add, eng="pool")
            if STOP <= 3:
                continue
            q_ = qkT[i % 2]
            for b0 in range(0, 32, 8):
                for jj in range(8):
                    P.transpose(pT[:96, jj, :], qb[:, b0 + jj, :], self.identb[:])
                P.copy(q_[:, b0:b0 + 8, :], pT[:96, :, :], eng=("dve" if (b0 // 8) % 2 == 0 else "act"))
            P.dma(R(self.QT, self.QT.h[:, :, i * 128:(i + 1) * 128].rearrange("m d t -> d m t")), q_[:, 0:16, :], key="qo%d" % (i % 2))
            P.dma(R(self.KT, self.KT.h[:, :, i * 128:(i + 1) * 128].rearrange("m d t -> d m t")), q_[:, 16:32, :], key="qo%d" % (i % 2))
        P.pop()
        import os
        if os.environ.get("MK_DUMP"):
            for nm, src in (("dQT", self.QT), ("dKT", self.KT)):
                dd = P.dout(nm, [16, 96, T], BF16)
                for m in range(16):
                    P.dma(dd[m], src[m], key="dump")
            dd = P.dout("dV", [T, 16, 65], BF16)
            for i in range(NT):
                P.dma(dd[i * 128:(i + 1) * 128], self.V[i * 128:(i + 1) * 128], key="dump")
            self.P.barrier()
            return
        if "attn" not in os.environ.get("MK_SKIP", ""):
            attn_core(P, C, self.QT, self.KT, self.V, self.O, 16, 96, lambda m: [m], lambda m: [m], 96 ** -0.5, self.identf)
        P.push()
        S = self.post_alloc(l, "mla_wout%d" % j)
        ot = [P.tile([128, 16, 65], F32, "ot") for _ in range(2)]
        rd = P.tile([128, 16], F32, "rd")
        ob = [P.tile([128, 1024], BF16, "ob") for _ in range(2)]
        for i in range(NT):
            o_ = ot[i % 2]
            P.dma(o_[:], self.O[i * 128:(i + 1) * 128, 0:16, :], key="fo%d" % (i % 2))
            P.recip(rd[:], o_[:, :, 64])
            o2 = ob[i % 2]
            P.tt(R(o2, o2.h[:].rearrange("p (h d) -> p h d", d=64)), o_[:, :, 0:64], R(rd, rd.h[:].unsqueeze(2).broadcast_to([128, 16, 64])), ALU.mult)
            self.post(l, S, i, o2)
        P.pop()


    def gdn_layer(self, l, j):
        import os
        P, C = self.P, self.C
        T, NT, CTX = C["T"], self.NT, C["CTX"]
        NC = T // 64
        ncc = CTX // 64
        if "PRE" not in self.__dict__:
            self.PRE = P.dscratch("PRE", [24, 128, T], F32)
            self.QKVT = P.dscratch("QKVT", [24, 128, T], BF16)
            self.Zd = P.dscratch("Zd", [T, 1024], F32)
            self.GB = P.dscratch("GB", [T, 32], F32)
            self.OG = P.dscratch("OG", [2, T, 1024], F32)
        PRE, QKVT, Zd, GB, OG = self.PRE, self.QKVT, self.Zd, self.GB, self.OG
        P.push()
        win_d = P.din("gdn_win%d" % j, [1024, 4128])
        alog_d = P.din("gdn_alog%d" % j, [16])
        dtb_d = P.din("gdn_dtb%d" % j, [16])
        w = P.tile([128, 8, 4128], BF16, "gwin")
        stg = [P.tile([128, 2048], F32, "stg") for _ in range(2)]
        self.load_w(w, win_d.h, 8, 4128, stg, [0])
        md = self.load_mod([0, 1])
        nega = P.tile([128, 16], F32, "nega")
        P.dma(nega[:], R(alog_d, alog_d.h[:].partition_broadcast(128)), key="g0", q="pool")
        P.act(nega[:], nega[:], AF.Exp)
        P.ts(nega[:], nega[:], -1.0, None, ALU.mult)
        dtb = P.tile([128, 16], F32, "dtb")
        P.dma(dtb[:], R(dtb_d, dtb_d.h[:].partition_broadcast(128)), key="g0", q="pool")
        xt = [P.tile([128, 1024], F32, "xt") for _ in range(2)]
        sq = P.tile([128, 1024], F32, "sq")
        tmp = P.tile([128, 1024], F32, "tmp")
        hb = P.tile([128, 1024], BF16, "hb")
        hT = [P.tile([128, 8, 128], BF16, "hT") for _ in range(2)]
        small = [(P.tile([128, 1], F32, "ss"), P.tile([128, 1], F32, "rs")) for _ in range(2)]
        pre = [P.tile([128, 24, 128], F32, "pre") for _ in range(1)]
        zt = [P.tile([128, 1024], F32, "zt") for _ in range(1)]
        gb = [P.tile([128, 32], F32, "gb") for _ in range(2)]
        t16 = P.tile([128, 16], F32, "t16")
        pT = P.psum([128, 8, 128], BF16, "pT")
        pf = [P.psum([128, 4, 128], F32, "pf") for _ in range(2)]
        pz = P.psum([128, 3, 512], F32, "pz")
        X = self.xsrc(l)
        for i in range(NT):
            which = 0 if i < self.nctx else 1
            x_ = xt[i % 2]
            P.dma(x_[:], X[i * 128:(i + 1) * 128, :], key="x%d" % (i % 2))
            self.front(x_, md[(which, 0)][:], md[(which, 1)][:], sq, small[i % 2], tmp, hb)
            h_ = hT[i % 2]
            self.transposeT(h_, hb, 8, pT)
            pr_ = pre[0]
            for g4 in range(6):
                pf_ = pf[g4 % 2]
                for c4 in range(4):
                    cc = g4 * 4 + c4
                    for k in range(8):
                        P.mm(pf_[:, c4, :], w[:, k, cc * 128:(cc + 1) * 128], h_[:, k, :], start=(k == 0), stop=(k == 7))
                P.copy(pr_[:, g4 * 4:(g4 + 1) * 4, :], pf_[:], eng=("act" if g4 % 2 == 0 else "dve"))
            P.dma(R(PRE, PRE.h[:, :, i * 128:(i + 1) * 128].rearrange("c p t -> p c t")), pr_[:], key="pre%d" % (i % 2))
            for n in range(2):
                for k in range(8):
                    P.mm(pz[:, n, :], h_[:, k, :], w[:, k, 3072 + n * 512:3072 + (n + 1) * 512], start=(k == 0), stop=(k == 7))
            for k in range(8):
                P.mm(pz[:, 2, 0:32], h_[:, k, :], w[:, k, 4096:4128], start=(k == 0), stop=(k == 7))
            z_ = zt[0]
            P.copy(R(z_, z_.h[:].rearrange("p (n f) -> p n f", n=2)), pz[:, 0:2, :], eng="act")
            P.dma(Zd[i * 128:(i + 1) * 128, :], z_[:], key="zo%d" % (i % 2), q="pool")
            g_ = gb[i % 2]
            P.tt(t16[:], pz[:, 2, 0:16], dtb[:], ALU.add)
            P.act(t16[:], t16[:], AF.Exp)
            P.ts(t16[:], t16[:], 1.0, None, ALU.add)
            P.act(t16[:], t16[:], AF.Ln)
            P.tt(g_[:, 0:16], t16[:], nega[:], ALU.mult)
            P.act(g_[:, 16:32], pz[:, 2, 16:32], AF.Exp, scale=-1.0)
            P.ts(g_[:, 16:32], g_[:, 16:32], 1.0, None, ALU.add)
            P.act(g_[:, 16:32], g_[:, 16:32], AF.Ln)
            P.ts(g_[:, 16:32], g_[:, 16:32], -1.0, None, ALU.mult)
            P.dma(GB[i * 128:(i + 1) * 128, :], g_[:], key="go%d" % (i % 2), q="pool")
        P.pop()
        P.push()
        cw_d = P.din("gdn_cw%d" % j, [128, 24, 5])
        cw = P.tile([128, 24, 5], F32, "cw")
        P.dma(cw[:], cw_d[:], key="g0")
        onesb = P.tile([128, 128], BF16, "onesb")
        P.memset(onesb[:], 1.0)
        xt = P.tile([128, T], F32, "cx")
        acc = P.tile([128, T], F32, "cacc")
        sqb = P.tile([128, T], BF16, "csq")
        ob = [P.tile([128, T], BF16, "cob") for _ in range(2)]
        rsb = [P.tile([128, 512], F32, "crs") for _ in range(2)]
        pss = [P.psum([128, 512], F32, "pss") for _ in range(2)]
        segs = [(0, CTX), (CTX, T)]
        for cc in range(24):
            P.dma(xt[:], PRE[cc], key="cx")
            P.ts(acc[:], xt[:], cw[:, cc, 2:3], None, ALU.mult)
            for off in (-2, -1, 1, 2):
                for (s0, s1) in segs:
                    a = max(s0, s0 - off)
                    b = min(s1, s1 - off)
                    P.stt(acc[:, a:b], xt[:, a + off:b + off], cw[:, cc, off + 2:off + 3], acc[:, a:b], ALU.mult, ALU.add)
            P.act(acc[:], acc[:], AF.Silu)
            o_ = ob[cc % 2]
            if cc < 16:
                P.act(sqb[:], acc[:], AF.Square)
                nb = 0
                for b0 in range(0, T, 512):
                    bw = min(512, T - b0)
                    ps_ = pss[nb % 2]
                    r_ = rsb[nb % 2]
                    nb += 1
                    P.mm(ps_[:, :bw], onesb[:], sqb[:, b0:b0 + bw])
                    P.ts(r_[:, :bw], ps_[:, :bw], EPS, None, ALU.add)
                    P.act(r_[:, :bw], r_[:, :bw], AF.Sqrt)
                    P.recip(r_[:, :bw], r_[:, :bw])
                    if cc < 8:
                        P.stt(o_[:, b0:b0 + bw], acc[:, b0:b0 + bw], 128 ** -0.5, r_[:, :bw], ALU.mult, ALU.mult)
                    else:
                        P.tt(o_[:, b0:b0 + bw], acc[:, b0:b0 + bw], r_[:, :bw], ALU.mult, eng="pool")
            else:
                P.copy(o_[:], acc[:], eng="pool")
            P.dma(QKVT[cc], o_[:], key="co%d" % (cc % 2), q="pool")
        P.pop()
        P.push()
        gm_d = P.din("gmask", [2, 4, 64, 64])
        idr_d = P.din("identrep", [64, 8, 64])
        identrep = P.tile([64, 8, 64], F32, "identrep")
        P.dma(identrep[:], idr_d[:], key="g0")
        ones64 = P.tile([64, 128], F32, "ones64")
        P.memset(ones64[:], 1.0)
        banks = [P.psum([128, 512], F32, "bank") for _ in range(7)]
        bankb = P.psum([128, 1024], BF16, "bankb")
        bi = [0]

        def nbank():
            b = banks[bi[0] % 7]
            bi[0] += 1
            return b
        gtok = P.tile([64, NC, 32], F32, "gtok")
        P.dma(gtok[:], R(GB, GB.h.rearrange("(c p) f -> p c f", p=64)), key="g1")
        S = P.tile([128, 8, 128], F32, "S")
        Sb = P.tile([128, 8, 128], BF16, "Sb")
        kq = [P.tile([128, 8, 2, 64], BF16, "kq") for _ in range(2)]
        vT = [P.tile([128, 8, 64], BF16, "vT") for _ in range(2)]
        dcol = P.tile([64, NC, 8], F32, "dcol")
        dbcol = P.tile([64, NC, 8], F32, "dbcol")
        ed = P.tile([64, NC, 8], F32, "ed")
        be = P.tile([64, NC, 8], F32, "be")
        ed2 = P.tile([64, NC, 8], F32, "ed2")
        bet = P.tile([64, NC, 8], F32, "bet")
        lastbc = P.tile([128, NC, 8], F32, "lastbc")
        Lm = P.tile([64, 64], F32, "Lm")
        negi = P.tile([64, 64], F32, "negi")
        negs = P.tile([64, 64], F32, "negs")
        poss = P.tile([64, 64], F32, "poss")
        Dg = P.tile([64, 2, 8, 64], F32, "Dg")
        dfa = P.tile([64, 8, 64], F32, "dfa")
        dfb = P.tile([64, 8, 64], F32, "dfb")
        dfc = P.tile([64, 8, 64], F32, "dfc")
        kkq = P.tile([64, 8, 128], F32, "kkq")
        X_ = [P.tile([64, 8, 64], F32, "X") for _ in range(2)]
        Y_ = [P.tile([64, 8, 64], F32, "Y") for _ in range(2)]
        Z_ = [P.tile([64, 8, 64], F32, "Z") for _ in range(2)]
        qkm = P.tile([64, 8, 64], BF16, "qkm")
        ktok = P.tile([64, 8, 128], BF16, "ktok")
        kdec = P.tile([64, 8, 128], BF16, "kdec")
        bv = P.tile([64, 8, 128], F32, "bv")
        rr = P.tile([64, 4, 128], F32, "rr")
        vn = P.tile([64, 4, 128], BF16, "vn")
        ot = [P.tile([64, 8, 128], F32, "ot") for _ in range(2)]
        o1s = P.tile([64, 4, 128], F32, "o1s")

        def b3(t, n):
            return R(t[0], t[0].h[:, t[1], :].unsqueeze(2).broadcast_to([64, 8, n]))

        def v3(bank, p, a, b):
            return R(bank, bank.h[:p, :a * b].rearrange("p (a b) -> p a b", a=a))
        for d in range(2):
            P.dma(Lm[:], gm_d[d, 0], key="g2")
            P.dma(negi[:], gm_d[d, 1], key="g2")
            P.dma(negs[:], gm_d[d, 2], key="g2")
            P.dma(poss[:], gm_d[d, 3], key="g2")
            gsl = R(gtok, gtok.h[:, :, d * 8:(d + 1) * 8])
            lbs = R(gtok, gtok.h[:, :, 16 + d * 8:16 + (d + 1) * 8])
            gflat = P.tile([64, NC, 8], F32, "gflat") if d == 0 else gflat
            P.copy(gflat[:], gsl)
            gf2 = gflat.h[:].rearrange("p c h -> p (c h)")
            for n0 in range(0, NC * 8, 512):
                nw = min(512, NC * 8 - n0)
                bk = nbank()
                P.mm(bk[:64, :nw], Lm[:], R(gflat, gf2[:, n0:n0 + nw]))
                P.copy(R(dcol, dcol.h[:].rearrange("p c h -> p (c h)")[:, n0:n0 + nw]), bk[:64, :nw])
                bk = nbank()
                P.mm(bk[:, :nw], ones64[:], R(gflat, gf2[:, n0:n0 + nw]))
                P.copy(R(lastbc, lastbc.h[:].rearrange("p c h -> p (c h)")[:, n0:n0 + nw]), bk[:, :nw], eng="act")
            P.tt(ed2[:], lastbc[:64], dcol[:], ALU.subtract)
            P.act(ed2[:], ed2[:], AF.Exp)
            P.act(lastbc[:], lastbc[:], AF.Exp)
            P.act(ed[:], dcol[:], AF.Exp)
            P.tt(dbcol[:], dcol[:], lbs, ALU.add)
            P.act(be[:], dbcol[:], AF.Exp)
            P.act(bet[:], lbs, AF.Exp)
            P.memset(S[:], 0.0)
            P.memset(Sb[:], 0.0)
            if d == 0:
                order = list(range(NC))
            else:
                order = list(range(ncc - 1, -1, -1)) + list(range(NC - 1, ncc - 1, -1))
            for ci, c in enumerate(order):
                t0 = c * 64
                kq_ = kq[ci % 2]
                v_ = vT[ci % 2]
                P.dma(kq_[:, :, 0, :], R(QKVT, QKVT.h[8:16, :, t0:t0 + 64].rearrange("h p t -> p h t")), key="lk%d" % (ci % 2))
                P.dma(kq_[:, :, 1, :], R(QKVT, QKVT.h[0:8, :, t0:t0 + 64].rearrange("h p t -> p h t")), key="lk%d" % (ci % 2))
                P.dma(v_[:], R(QKVT, QKVT.h[16:24, :, t0:t0 + 64].rearrange("h p t -> p h t")), key="lv%d" % (ci % 2), q="pool")
                for hg in range(2):
                    bk = nbank()
                    for h4 in range(4):
                        h = hg * 4 + h4
                        P.mm(bk[:64, h4 * 128:(h4 + 1) * 128], kq_[:, h, 0, :], R(kq_, kq_.h[:, h, :, :].rearrange("p a t -> p (a t)")))
                    P.copy(kkq[:, hg * 4:(hg + 1) * 4, :], v3(bk, 64, 4, 128), eng="act")
                P.tt(Dg[:, 0], identrep[:], b3((dcol, c), 64), ALU.mult)
                P.tt(Dg[:, 1], identrep[:], b3((dbcol, c), 64), ALU.mult, eng="pool")
                bk0 = nbank()
                P.mm(bk0[:64, :], ones64[:, :64], R(Dg, Dg.h[:, 0].rearrange("p h i -> p (h i)")))
                bk1 = nbank()
                P.mm(bk1[:64, :], ones64[:, :64], R(Dg, Dg.h[:, 1].rearrange("p h i -> p (h i)")))
                P.tt(dfa[:], v3(bk0, 64, 8, 64), b3((dcol, c), 64), ALU.subtract)
                P.tt(dfa[:], dfa[:], R(negi, negi.h[:].unsqueeze(1).broadcast_to([64, 8, 64])), ALU.add, eng="pool")
                P.act(dfa[:], dfa[:], AF.Exp)
                P.tt(qkm[:], kkq[:, :, 64:128], dfa[:], ALU.mult)
                P.tt(dfb[:], v3(bk1, 64, 8, 64), b3((dcol, c), 64), ALU.subtract)
                P.tt(dfb[:], dfb[:], R(negs, negs.h[:].unsqueeze(1).broadcast_to([64, 8, 64])), ALU.add, eng="pool")
                P.act(dfb[:], dfb[:], AF.Exp)
                X0 = X_[0]
                P.stt(X0[:], kkq[:, :, 0:64], -1.0, dfb[:], ALU.mult, ALU.mult)
                P.tt(dfc[:], v3(bk0, 64, 8, 64), b3((dbcol, c), 64), ALU.subtract)
                P.tt(dfc[:], dfc[:], R(poss, poss.h[:].unsqueeze(1).broadcast_to([64, 8, 64])), ALU.add, eng="pool")
                P.act(dfc[:], dfc[:], AF.Exp, scale=-1.0)
                Y0 = Y_[0]
                P.stt(Y0[:], kkq[:, :, 0:64], -1.0, dfc[:], ALU.mult, ALU.mult)
                Zc = Z_[0]
                P.tt(Zc[:], X0[:], identrep[:], ALU.add, eng="pool")
                Xc, Yc = X0, Y0
                for lev in range(1, 6):
                    Yn = Y_[lev % 2]
                    bk = nbank()
                    for h in range(8):
                        P.mm(bk[:64, h * 64:(h + 1) * 64], Xc[:, h, :], Yc[:, h, :])
                    if lev < 5:
                        Xn = X_[lev % 2]
                        bk2 = nbank()
                        for h in range(8):
                            P.mm(bk2[:64, h * 64:(h + 1) * 64], Yc[:, h, :], Xc[:, h, :])
                        P.copy(Xn[:], v3(bk2, 64, 8, 64), eng="act")
                    P.copy(Yn[:], v3(bk, 64, 8, 64))
                    Zn = Z_[lev % 2]
                    bk3 = nbank()
                    for h in range(8):
                        P.mm(bk3[:64, h * 64:(h + 1) * 64], Yn[:, h, :], Zc[:, h, :])
                    P.tt(Zn[:], Zc[:], v3(bk3, 64, 8, 64), ALU.add)
                    Zc = Zn
                    Yc = Yn
                    if lev < 5:
                        Xc = Xn
                for h in range(8):
                    P.transpose(bankb[:64, h * 128:(h + 1) * 128], kq_[:, h, 0, :], self.identb[:])
                kt3 = R(bankb, bankb.h[:64, :].rearrange("p (h d) -> p h d", h=8))
                P.tt(kdec[:], kt3, b3((ed2, c), 128), ALU.mult)
                for h in range(8):
                    P.transpose(bankb[:64, h * 128:(h + 1) * 128], v_[:, h, :], self.identb[:])
                P.tt(bv[:], kt3, b3((bet, c), 128), ALU.mult)
                o_ = ot[ci % 2]
                for hg in range(2):
                    hs = slice(hg * 4, (hg + 1) * 4)
                    bk = nbank()
                    for h4 in range(4):
                        h = hg * 4 + h4
                        P.mm(bk[:64, h4 * 128:(h4 + 1) * 128], kq_[:, h, 0, :], Sb[:, h, :])
                    be4 = R(be, be.h[:, c, hs].unsqueeze(2).broadcast_to([64, 4, 128]))
                    P.tt(rr[:], v3(bk, 64, 4, 128), be4, ALU.mult)
                    P.tt(rr[:], bv[:, hs, :], rr[:], ALU.subtract, eng="pool")
                    bk = nbank()
                    for h4 in range(4):
                        h = hg * 4 + h4
                        P.mm(bk[:64, h4 * 128:(h4 + 1) * 128], Zc[:, h, :], rr[:, h4, :])
                    P.copy(vn[:], v3(bk, 64, 4, 128), eng="act")
                    bk = nbank()
                    for h4 in range(4):
                        h = hg * 4 + h4
                        P.mm(bk[:64, h4 * 128:(h4 + 1) * 128], kq_[:, h, 1, :], Sb[:, h, :])
                    ed4 = R(ed, ed.h[:, c, hs].unsqueeze(2).broadcast_to([64, 4, 128]))
                    P.tt(o1s[:], v3(bk, 64, 4, 128), ed4, ALU.mult)
                    bk = nbank()
                    for h4 in range(4):
                        h = hg * 4 + h4
                        P.mm(bk[:64, h4 * 128:(h4 + 1) * 128], qkm[:, h, :], vn[:, h4, :])
                    P.tt(o_[:, hs, :], o1s[:], v3(bk, 64, 4, 128), ALU.add)
                    bk = nbank()
                    for h4 in range(4):
                        h = hg * 4 + h4
                        P.mm(bk[:, h4 * 128:(h4 + 1) * 128], kdec[:, h, :], vn[:, h4, :])
                    l4 = R(lastbc, lastbc.h[:, c, hs].unsqueeze(2).broadcast_to([128, 4, 128]))
                    P.tt(S[:, hs, :], S[:, hs, :], l4, ALU.mult, eng="pool")
                    P.tt(S[:, hs, :], S[:, hs, :], v3(bk, 128, 4, 128), ALU.add)
                    P.copy(Sb[:, hs, :], S[:, hs, :], eng="act")
                P.dma(OG[d, t0:t0 + 64, :], R(o_, o_.h[:].rearrange("p h d -> p (h d)")), key="og%d" % (ci % 2))
        P.pop()
        P.push()
        S2 = self.post_alloc(l, "gdn_wout%d" % j)
        og_d = P.din("gdn_og%d" % j, [128])
        ogn = P.tile([128, 8, 128], F32, "ogn")
        P.dma(ogn[:], R(og_d, og_d.h[:].partition_broadcast(128).unsqueeze(1).broadcast_to([128, 8, 128])), key="fl")
        ot2 = [P.tile([128, 8, 128], F32, "ot2") for _ in range(2)]
        zt2 = [P.tile([128, 1024], F32, "zt2") for _ in range(2)]
        sqd = P.tile([128, 8, 128], F32, "sqd")
        ss8 = P.tile([128, 8], F32, "ss8")
        rs8 = P.tile([128, 8], F32, "rs8")
        ob2 = [P.tile([128, 1024], BF16, "ob2") for _ in range(2)]
        for i in range(NT):
            o_ = ot2[i % 2]
            z_ = zt2[i % 2]
            P.dma(R(o_, o_.h[:].rearrange("p h d -> p (h d)")), OG[0, i * 128:(i + 1) * 128, :], key="fo%d" % (i % 2))
            P.dma(R(sqd, sqd.h[:].rearrange("p h d -> p (h d)")), OG[1, i * 128:(i + 1) * 128, :], key="fo%d" % (i % 2))
            P.dma(z_[:], Zd[i * 128:(i + 1) * 128, :], key="fz%d" % (i % 2), q="pool")
            P.tt(o_[:], o_[:], sqd[:], ALU.add, eng="pool")
            P.act(sqd[:], o_[:], AF.Square)
            P.reduce(ss8[:], sqd[:])
            self.rstd(rs8[:], ss8[:], 128)
            P.tt(o_[:], o_[:], R(rs8, rs8.h[:].unsqueeze(2).broadcast_to([128, 8, 128])), ALU.mult)
            P.tt(o_[:], o_[:], ogn[:], ALU.mult, eng="pool")
            P.act(z_[:], z_[:], AF.Silu)
            o2 = ob2[i % 2]
            P.tt(o2[:], R(o_, o_.h[:].rearrange("p h d -> p (h d)")), z_[:], ALU.mult)
            self.post(l, S2, i, o2)
        P.pop()


def rope_tables(T, CTX, GRID_W, dim):
    nf = dim // 4
    inv = 10000.0 ** (-np.arange(nf, dtype=np.float32) / nf)
    n = T - CTX
    rows = n // GRID_W
    r = np.repeat(np.arange(rows, dtype=np.float32), GRID_W)
    c = np.tile(np.arange(GRID_W, dtype=np.float32), rows)
    ang = np.concatenate([r[:, None] * inv, c[:, None] * inv], axis=-1).astype(np.float32)
    cos = np.ones((T, dim // 2), np.float32)
    sin = np.zeros((T, dim // 2), np.float32)
    cos[CTX:] = np.cos(ang)
    sin[CTX:] = np.sin(ang)
    return cos, sin


def build_program(C):
    P = Prog()
    net = Net(P, C)
    import os
    for l in range(int(os.environ.get("MK_L0", "0")), C["DEPTH"]):
        last = l == C["DEPTH"] - 1
        j = l // 3
        kind = l % 3
        import os
        if os.environ.get("MK_ALLDA"):
            kind, j = 0, 0
        net.mod_phase(l)
        if kind == 0:
            net.da_proj(l, j)
            attn_core(P, C, net.QT, net.KT, net.V, net.O, 16, 64,
                      lambda m: [2 * (m // 2), 2 * (m // 2) + 1], lambda m: [2 * m, 2 * m + 1], 0.125, net.identf)
            net.da_finish(l, j)
        elif kind == 1:
            net.mla_layer(l, j)
        else:
            net.gdn_layer(l, j)
        if os.environ.get("MK_DUMP") and l == 1:
            break
        net.moe_phase(l, last)
    return P, net


def host_inputs(C, inp, b):
    T, CTX, SEQ = C["T"], C["CTX"], C["SEQ"]
    NT = T // 128
    f32 = np.float32
    d = {}
    d["ident"] = np.eye(128, dtype=f32)
    d["xall"] = np.ascontiguousarray(np.concatenate([inp["ctx"][b], inp["x"][b]], axis=0))
    cc = np.stack([inp["c"][b], inp["c_ctx"]], axis=-1)
    d["cT"] = np.ascontiguousarray(cc.reshape(8, 128, 2).transpose(1, 0, 2))
    cosD, sinD = rope_tables(T, CTX, C["GRID_W"], 64)
    d["cosD"], d["sinD"] = cosD, sinD
    cosM, sinM = rope_tables(T, CTX, C["GRID_W"], 32)
    d["cosM"], d["sinM"] = cosM, sinM
    U = np.triu(np.ones((128, 128), f32), 1)
    d["U"] = U
    d["tokidx"] = (np.arange(NT)[None, :] * 128 + np.arange(128)[:, None]).astype(f32)
    caps = [2 * CTX // 16, 2 * SEQ // 16]
    ecap = np.zeros((128, 2, 16), f32)
    base = 0
    for si in range(2):
        ecap[:, si, :] = base + np.arange(16) * caps[si]
        base += 16 * caps[si]
    d["ecap"] = ecap
    ipc = np.zeros((128, NT, 2), f32)
    ipc[:, :, 0] = np.arange(NT)[None, :]
    ipc[:, :, 1] = np.arange(128)[:, None]
    d["ipc"] = ipc
    d["iota"] = np.ascontiguousarray(np.broadcast_to(np.arange(1024, dtype=f32)[None, :], (128, 1024)))
    ii = np.arange(64)
    gm = np.zeros((2, 4, 64, 64), f32)
    NEG = -1.0e9
    le = (ii[:, None] <= ii[None, :]); lt = (ii[:, None] < ii[None, :])
    ge = (ii[:, None] >= ii[None, :]); gt = (ii[:, None] > ii[None, :])
    gm[0, 0] = le; gm[0, 1] = np.where(le, 0, NEG); gm[0, 2] = np.where(lt, 0, NEG); gm[0, 3] = np.where(gt, 0, -NEG)
    gm[1, 0] = ge; gm[1, 1] = np.where(ge, 0, NEG); gm[1, 2] = np.where(gt, 0, NEG); gm[1, 3] = np.where(lt, 0, -NEG)
    d["gmask"] = gm
    d["identrep"] = np.ascontiguousarray(np.broadcast_to(np.eye(64, dtype=f32)[:, None, :], (64, 8, 64)))
    d["dump"] = (base + np.arange(128)).astype(f32).reshape(128, 1)
    for l in range(C["DEPTH"]):
        d["ada_w%d" % l] = inp["ada_w"][l]
        d["ada_b%d" % l] = inp["ada_b"][l]
        d["ng%d" % l] = inp["norm_g"][l]
        d["rt%d" % l] = inp["moe_router"][l]
        d["wg%d" % l] = inp["moe_w_gate"][l]
        d["wu%d" % l] = inp["moe_w_up"][l]
        d["wd%d" % l] = inp["moe_w_down"][l]
        j = l // 3
        kind = l % 3
        import os
        if os.environ.get("MK_ALLDA"):
            kind, j = 0, 0
        if kind == 0:
            d["da_win%d" % j] = inp["da_w_in"][j]
            d["da_qg%d" % j] = inp["da_q_gain"][j]
            d["da_kg%d" % j] = inp["da_k_gain"][j]
            d["da_lam%d" % j] = np.ascontiguousarray(inp["da_lambda"][j].reshape(256))
            d["da_sg%d" % j] = inp["da_sub_gain"][j]
            d["da_wout%d" % j] = inp["da_w_out"][j]
        elif kind == 2:
            d["gdn_win%d" % j] = inp["gdn_w_in"][j]
            d["gdn_alog%d" % j] = np.ascontiguousarray(inp["gdn_a_log"][j].reshape(16))
            d["gdn_dtb%d" % j] = np.ascontiguousarray(inp["gdn_dt_bias"][j].reshape(16))
            d["gdn_cw%d" % j] = np.ascontiguousarray(inp["gdn_conv_w"][j].reshape(5, 24, 128).transpose(2, 1, 0))
            d["gdn_og%d" % j] = inp["gdn_o_gain"][j]
            d["gdn_wout%d" % j] = inp["gdn_w_out"][j]
        if kind == 1:
            d["mla_wdown%d" % j] = inp["mla_w_down"][j]
            d["mla_wuq%d" % j] = inp["mla_w_uq"][j]
            d["mla_wukv%d" % j] = inp["mla_w_ukv"][j]
            d["mla_qag%d" % j] = inp["mla_q_a_gain"][j]
            d["mla_kvag%d" % j] = inp["mla_kv_a_gain"][j]
            d["mla_qg%d" % j] = inp["mla_q_gain"][j]
            d["mla_kg%d" % j] = inp["mla_k_gain"][j]
            d["mla_wout%d" % j] = inp["mla_w_out"][j]
    return d


def kernel(**inp):
    inp = {k: np.asarray(v) for k, v in inp.items()}
    B, SEQ, D = inp["x"].shape
    CTX = inp["ctx"].shape[1]
    C = dict(SEQ=SEQ, CTX=CTX, T=SEQ + CTX, GRID_W=64, DEPTH=inp["ada_w"].shape[0], FF=inp["moe_w_gate"].shape[-1])
    P, net = build_program(C)
    nc = P.build()
    names = set()
    import concourse.mybir as mb
    in_maps = []
    for b in range(B):
        d = host_inputs(C, inp, b)
        in_maps.append(d)
    used = [a.memorylocations[0].name for a in nc.allocations if isinstance(a, mb.MemoryLocationSet) and a.kind == "ExternalInput"]
    in_maps = [{k: np.ascontiguousarray(m[k]) for k in used if k in m} for m in in_maps]
    res = run_bass_kernel_spmd(nc, in_maps, core_ids=list(range(B)))
    import os
    if os.environ.get("MK_DUMP"):
        return res.results
    return np.stack([res.results[b]["out"] for b in range(B)], axis=0).astype(np.float32)
```
